# Optimizing a Trainium2 kernel written in Bass

```python
import numpy as np
import jax
import jax.numpy as jnp
from jax import lax

D_MODEL = 1024
BATCH = 8
SEQ = 4096
DEPTH = 2

D_MIX = D_MODEL
NSA_HEADS = 8
NSA_KV_GROUPS = 2
NSA_HEAD_DIM = 64
NSA_WIDTH = NSA_HEADS * NSA_HEAD_DIM
NSA_KV_WIDTH = NSA_KV_GROUPS * NSA_HEAD_DIM
CMP_BLOCK = 32
CMP_STRIDE = 16
CMP_HIDDEN = 2 * NSA_HEAD_DIM
SLC_BLOCK = 64
SLC_TOPN = 16
WINDOW = 512
Q_BLOCK = 64
FORCE_BONUS = 1e4
M_HEADS = 4
M_WIDTH = D_MIX - NSA_WIDTH
M_HEAD_DIM = M_WIDTH // M_HEADS
M_CHUNK = 64
CONV_WIDTH = 4
FFN_HIDDEN = ((8 * D_MODEL + 3 * 256 - 1) // (3 * 256)) * 256
D_IN_PROJ = NSA_WIDTH + 6 * NSA_KV_WIDTH + 3 * NSA_HEADS + 4 * M_WIDTH + 2 * M_HEADS
RMS_EPS = 1e-6
NEG_INF = -1e30

kernel_name = "hybrid_nsa_mlstm_block"


def rmsnorm(x, g):
    xf = x.astype(jnp.float32)
    y = xf * lax.rsqrt(jnp.mean(xf * xf, axis=-1, keepdims=True) + RMS_EPS)
    return (y * g.astype(jnp.float32)).astype(x.dtype)


def in_proj_sizes():
    return ([NSA_WIDTH] + [NSA_KV_WIDTH] * 6 + [3 * NSA_HEADS]
            + [M_WIDTH] * 3 + [M_HEADS, M_HEADS, M_WIDTH])


def compress_blocks(a, pe, w1, w2):
    B, G, T, dh = a.shape
    ac = a.reshape(B, G, T // CMP_STRIDE, CMP_STRIDE, dh)
    blocks = jnp.concatenate([ac[:, :, :-1], ac[:, :, 1:]], axis=3) + pe
    flat = blocks.reshape(B, G, T // CMP_STRIDE - 1, CMP_BLOCK * dh)
    return jax.nn.silu(flat @ w1) @ w2


def nsa_mixer(q, kvs, gates, q_g, k_g, pe, w1, w2, out_g):
    B, T, _ = q.shape
    H, G, dh = NSA_HEADS, NSA_KV_GROUPS, NSA_HEAD_DIM
    HG = H // G
    scale = dh ** -0.5
    q = rmsnorm(q.reshape(B, T, G, HG, dh), q_g).transpose(0, 2, 3, 1, 4)
    k_c, v_c, k_s, v_s, k_w, v_w = [a.reshape(B, T, G, dh).transpose(0, 2, 1, 3) for a in kvs]
    k_s = rmsnorm(k_s, k_g[1])
    k_w = rmsnorm(k_w, k_g[2])
    t_pos = jnp.arange(T)

    k_cmp = rmsnorm(compress_blocks(k_c, pe[0], w1[0], w2[0]), k_g[0])
    v_cmp = compress_blocks(v_c, pe[1], w1[1], w2[1])
    n_cmp = T // CMP_STRIDE - 1
    cmp_start = jnp.arange(n_cmp) * CMP_STRIDE
    cmp_end = cmp_start + CMP_BLOCK - 1
    cmp_valid = cmp_end[None, :] <= t_pos[:, None]
    s = jnp.einsum("bghtd,bgnd->bghtn", q, k_cmp).astype(jnp.float32) * scale
    p_cmp = jax.nn.softmax(jnp.where(cmp_valid, s, NEG_INF), axis=-1) * cmp_valid
    o_cmp = jnp.einsum("bghtn,bgnd->bghtd", p_cmp.astype(v_cmp.dtype), v_cmp)

    n_blk = T // SLC_BLOCK
    n_sel = min(SLC_TOPN, n_blk)
    blk = jnp.arange(n_blk)
    overlap = ((cmp_start[:, None] < (blk[None, :] + 1) * SLC_BLOCK)
               & (cmp_end[:, None] >= blk[None, :] * SLC_BLOCK)).astype(jnp.float32)
    imp = jnp.einsum("bghtn,nj->bgtj", p_cmp, overlap)
    cur = t_pos // SLC_BLOCK
    forced = (blk[None] == 0) | (blk[None] == cur[:, None]) | (blk[None] == cur[:, None] - 1)
    sel_valid = blk[None] * SLC_BLOCK <= t_pos[:, None]
    score = jnp.where(sel_valid, imp + FORCE_BONUS * forced.astype(jnp.float32), NEG_INF)
    _, sel_idx = lax.top_k(score, n_sel)

    n_qb = T // Q_BLOCK
    span = WINDOW + Q_BLOCK
    ks_blk = k_s.reshape(B, G, n_blk, SLC_BLOCK, dh)
    vs_blk = v_s.reshape(B, G, n_blk, SLC_BLOCK, dh)
    kw_pad = jnp.pad(k_w, ((0, 0), (0, 0), (WINDOW, 0), (0, 0)))
    vw_pad = jnp.pad(v_w, ((0, 0), (0, 0), (WINDOW, 0), (0, 0)))
    q_blocks = q.reshape(B, G, HG, n_qb, Q_BLOCK, dh).transpose(3, 0, 1, 2, 4, 5)
    idx_blocks = sel_idx.reshape(B, G, n_qb, Q_BLOCK, n_sel).transpose(2, 0, 1, 3, 4)
    gather = jax.vmap(jax.vmap(lambda kv_blk, ind: kv_blk[ind]))

    def step(args):
        c, qb, ib = args
        t = c * Q_BLOCK + jnp.arange(Q_BLOCK)
        k_sel = gather(ks_blk, ib)
        v_sel = gather(vs_blk, ib)
        pos = ib[..., None] * SLC_BLOCK + jnp.arange(SLC_BLOCK)
        msk = pos <= t[:, None, None]
        s_sel = jnp.einsum("bghqd,bgqnld->bghqnl", qb, k_sel).astype(jnp.float32) * scale
        s_sel = jnp.where(msk[:, :, None], s_sel, NEG_INF).reshape(B, G, HG, Q_BLOCK, n_sel * SLC_BLOCK)
        p_sel = jax.nn.softmax(s_sel, axis=-1)
        o_sel = jnp.einsum("bghqm,bgqmd->bghqd", p_sel.astype(v_sel.dtype),
                           v_sel.reshape(B, G, Q_BLOCK, n_sel * SLC_BLOCK, dh))
        k_win = lax.dynamic_slice_in_dim(kw_pad, c * Q_BLOCK, span, axis=2)
        v_win = lax.dynamic_slice_in_dim(vw_pad, c * Q_BLOCK, span, axis=2)
        s_pos = c * Q_BLOCK - WINDOW + jnp.arange(span)
        w_mask = ((s_pos[None] <= t[:, None]) & (s_pos[None] > t[:, None] - WINDOW)
                  & (s_pos[None] >= 0))
        s_w = jnp.einsum("bghqd,bgkd->bghqk", qb, k_win).astype(jnp.float32) * scale
        p_w = jax.nn.softmax(jnp.where(w_mask, s_w, NEG_INF), axis=-1)
        o_win = jnp.einsum("bghqk,bgkd->bghqd", p_w.astype(v_win.dtype), v_win)
        return o_sel, o_win

    o_sel, o_win = lax.map(step, (jnp.arange(n_qb), q_blocks, idx_blocks))
    o_sel = o_sel.transpose(1, 2, 3, 0, 4, 5).reshape(B, G, HG, T, dh)
    o_win = o_win.transpose(1, 2, 3, 0, 4, 5).reshape(B, G, HG, T, dh)

    g = jax.nn.sigmoid(gates).reshape(B, T, G, HG, 3).transpose(0, 2, 3, 1, 4)
    o = g[..., 0:1] * o_cmp + g[..., 1:2] * o_sel + g[..., 2:3] * o_win
    o = o.transpose(0, 3, 1, 2, 4).reshape(B, T, H, dh)
    return rmsnorm(o, out_g.reshape(H, dh)).reshape(B, T, NSA_WIDTH)


def causal_dwconv(u, w, b):
    C = u.shape[-1]
    y = lax.conv_general_dilated(u, w[:, None, :].astype(u.dtype), window_strides=(1,),
                                 padding=[(CONV_WIDTH - 1, 0)],
                                 dimension_numbers=("NWC", "WIO", "NWC"),
                                 feature_group_count=C)
    return y + b


def mlstm_mixer(q, k, v, i_pre, f_pre, o_pre, conv_w, conv_b, f_bias, out_g):
    B, T, _ = q.shape
    NH, DH, L = M_HEADS, M_HEAD_DIM, M_CHUNK
    n_ck = T // L
    f32 = jnp.float32
    qk = jax.nn.silu(causal_dwconv(jnp.concatenate([q, k], axis=-1), conv_w, conv_b))
    q, k = qk[..., :M_WIDTH], qk[..., M_WIDTH:]

    def heads(a):
        return a.astype(f32).reshape(B, n_ck, L, NH, DH).transpose(1, 0, 3, 2, 4)

    def gate(a):
        return a.astype(f32).reshape(B, n_ck, L, NH).transpose(1, 0, 3, 2)

    qh, kh, vh = heads(q), heads(k) * (DH ** -0.5), heads(v)
    log_f = gate(jax.nn.log_sigmoid(f_pre.astype(f32) + f_bias.astype(f32)))
    log_i = gate(i_pre)
    b_cum = jnp.cumsum(log_f, axis=-1)
    causal = jnp.tril(jnp.ones((L, L), dtype=bool))

    def chunk_step(carry, xs):
        C, n, m = carry
        qc, kc, vc, bc, ic = xs
        a = bc + m[..., None]
        d = jnp.where(causal, bc[..., :, None] - bc[..., None, :] + ic[..., None, :], NEG_INF)
        m_t = jnp.maximum(a, jnp.max(d, axis=-1))
        w_inter = jnp.exp(a - m_t)
        w_intra = jnp.exp(d - m_t[..., None]) * jnp.einsum("bhtd,bhsd->bhts", qc, kc)
        num = (w_inter[..., None] * jnp.einsum("bhtd,bhde->bhte", qc, C)
               + jnp.einsum("bhts,bhse->bhte", w_intra, vc))
        den = w_inter * jnp.einsum("bhtd,bhd->bht", qc, n) + jnp.sum(w_intra, axis=-1)
        h = num / jnp.maximum(jnp.abs(den), jnp.exp(-m_t))[..., None]
        b_last = bc[..., -1]
        g = b_last[..., None] - bc + ic
        m_new = jnp.maximum(b_last + m, jnp.max(g, axis=-1))
        decay = jnp.exp(b_last + m - m_new)
        w_s = jnp.exp(g - m_new[..., None])
        C_new = decay[..., None, None] * C + jnp.einsum("bhs,bhsd,bhse->bhde", w_s, kc, vc)
        n_new = decay[..., None] * n + jnp.einsum("bhs,bhsd->bhd", w_s, kc)
        return (C_new, n_new, m_new), h

    init = (jnp.zeros((B, NH, DH, DH), f32), jnp.zeros((B, NH, DH), f32), jnp.zeros((B, NH), f32))
    _, h = lax.scan(chunk_step, init, (qh, kh, vh, b_cum, log_i))
    h = h.transpose(1, 0, 3, 2, 4).reshape(B, T, NH, DH)
    h = rmsnorm(h, out_g.reshape(NH, DH)).reshape(B, T, M_WIDTH).astype(o_pre.dtype)
    return jax.nn.sigmoid(o_pre) * h


def setup_inputs(seed: int = 0) -> dict:
    key = jax.random.key(seed)
    ks = jax.random.split(key, 20)
    nrm = lambda k, shape, s: jax.random.normal(k, shape, jnp.float32) * s
    dh = NSA_HEAD_DIM
    return {
        "x": nrm(ks[0], (BATCH, SEQ, D_MODEL), 1.0),
        "ln1_g": 1.0 + nrm(ks[1], (DEPTH, D_MODEL), 0.02),
        "w_in": nrm(ks[2], (DEPTH, D_MODEL, D_IN_PROJ), D_MODEL ** -0.5),
        "b_in": nrm(ks[3], (DEPTH, D_IN_PROJ), 0.02),
        "nsa_q_norm_g": 1.0 + nrm(ks[4], (DEPTH, dh), 0.02),
        "nsa_k_norm_g": 1.0 + nrm(ks[5], (DEPTH, 3, dh), 0.02),
        "cmp_pe": nrm(ks[6], (DEPTH, 2, CMP_BLOCK, dh), 0.02),
        "cmp_w1": nrm(ks[7], (DEPTH, 2, CMP_BLOCK * dh, CMP_HIDDEN), (CMP_BLOCK * dh) ** -0.5),
        "cmp_w2": nrm(ks[8], (DEPTH, 2, CMP_HIDDEN, dh), CMP_HIDDEN ** -0.5),
        "m_conv_w": nrm(ks[9], (DEPTH, CONV_WIDTH, 2 * M_WIDTH), CONV_WIDTH ** -0.5),
        "m_conv_b": nrm(ks[10], (DEPTH, 2 * M_WIDTH), 0.02),
        "m_fgate_b": jnp.linspace(3.0, 6.0, M_HEADS, dtype=jnp.float32)[None, :] + nrm(ks[11], (DEPTH, M_HEADS), 0.1),
        "nsa_out_norm_g": 1.0 + nrm(ks[12], (DEPTH, NSA_WIDTH), 0.02),
        "m_out_norm_g": 1.0 + nrm(ks[13], (DEPTH, M_WIDTH), 0.02),
        "w_out": nrm(ks[14], (DEPTH, D_MIX, D_MODEL), D_MIX ** -0.5),
        "ln2_g": 1.0 + nrm(ks[15], (DEPTH, D_MODEL), 0.02),
        "w_gate_up": nrm(ks[16], (DEPTH, D_MODEL, 2 * FFN_HIDDEN), D_MODEL ** -0.5),
        "w_down": nrm(ks[17], (DEPTH, FFN_HIDDEN, D_MODEL), FFN_HIDDEN ** -0.5),
    }


def reference(x, ln1_g, w_in, b_in, nsa_q_norm_g, nsa_k_norm_g, cmp_pe, cmp_w1, cmp_w2,
              m_conv_w, m_conv_b, m_fgate_b, nsa_out_norm_g, m_out_norm_g, w_out,
              ln2_g, w_gate_up, w_down):
    offsets = np.cumsum(in_proj_sizes())[:-1].tolist()
    for l in range(DEPTH):
        h = rmsnorm(x, ln1_g[l])
        proj = h @ w_in[l] + b_in[l]
        (q_a, kc, vc, ksl, vsl, kwn, vwn, g_a,
         q_b, k_b, v_b, i_b, f_b, o_b) = jnp.split(proj, offsets, axis=-1)
        y_a = nsa_mixer(q_a, (kc, vc, ksl, vsl, kwn, vwn), g_a, nsa_q_norm_g[l], nsa_k_norm_g[l],
                        cmp_pe[l], cmp_w1[l], cmp_w2[l], nsa_out_norm_g[l])
        y_b = mlstm_mixer(q_b, k_b, v_b, i_b, f_b, o_b, m_conv_w[l], m_conv_b[l],
                          m_fgate_b[l], m_out_norm_g[l])
        x = x + jnp.concatenate([y_a, y_b.astype(y_a.dtype)], axis=-1) @ w_out[l]
        h = rmsnorm(x, ln2_g[l])
        gu = h @ w_gate_up[l]
        x = x + (jax.nn.silu(gu[..., :FFN_HIDDEN]) * gu[..., FFN_HIDDEN:]) @ w_down[l]
    return x
```

```python
import numpy as np
import concourse.bass as bass
import concourse.mybir as mybir
from concourse.bass_utils import run_bass_kernel_spmd
from contextlib import ExitStack

F32 = mybir.dt.float32
BF16 = mybir.dt.bfloat16
ALU = mybir.AluOpType
AF = mybir.ActivationFunctionType
AX = mybir.AxisListType

T = 4096
DM = 1024
DEPTH = 2
DIN = 3360
FF = 2816
NG = 8
EPS = 1e-6
NEG = -30000.0
NDMA_SEMS = 24

O_QA, O_KC, O_VC, O_KS, O_VS, O_KW, O_VW, O_GA = 0, 512, 640, 768, 896, 1024, 1152, 1280
O_QB, O_KB, O_VB, O_IF, O_OB = 1304, 1816, 2328, 2840, 2848


class Ctx:
    def __init__(self, nc, es):
        self.nc = nc
        self.engs = {"pe": nc.tensor, "act": nc.scalar, "dve": nc.vector, "pool": nc.gpsimd, "sp": nc.sync}
        self.sem = {k: es.enter_context(nc.semaphore("s_" + k)) for k in self.engs}
        self.cnt = {k: 0 for k in self.engs}
        self.seen = {k: {} for k in self.engs}
        self.dsem = [es.enter_context(nc.semaphore("s_dma%d" % i)) for i in range(NDMA_SEMS)]
        self.dcnt = [0] * NDMA_SEMS
        self.dnext = 0
        self.res = {}
        self.n_wait = 0
        self.n_ins = 0
        self.uid = 0

    def _semh(self, key):
        return self.sem[key] if isinstance(key, str) else self.dsem[key[1]]

    def _wait(self, eng, tok):
        key, val = tok
        if self.seen[eng].get(key, 0) >= val:
            return
        if key == eng and eng == "pe":
            return
        self.engs[eng].wait_ge(self._semh(key), val)
        self.seen[eng][key] = val
        self.n_wait += 1

    @staticmethod
    def _key(a):
        if isinstance(a, (str, tuple)):
            return a
        t = getattr(a, "tensor", None)
        return t.name if t is not None else a.name

    def _deps(self, eng, r, w):
        toks = []
        for a in r:
            ent = self.res.get(self._key(a))
            if ent and ent[0] is not None:
                toks.append(ent[0])
        for a in w:
            ent = self.res.get(self._key(a))
            if ent:
                if ent[0] is not None:
                    toks.append(ent[0])
                toks.extend(ent[1])
        for t in toks:
            self._wait(eng, t)

    def _record(self, tok, r, w):
        for a in r:
            ent = self.res.setdefault(self._key(a), [None, []])
            ent[1] = [t for t in ent[1] if t[0] != tok[0]] + [tok]
        for a in w:
            self.res[self._key(a)] = [tok, []]

    def op(self, eng, fn, r=(), w=()):
        self._deps(eng, r, w)
        ins = fn(self.engs[eng])
        self.cnt[eng] += 1
        ins.then_inc(self.sem[eng], 1)
        self._record((eng, self.cnt[eng]), r, w)
        self.n_ins += 1
        return ins

    def dma(self, out, in_, q="sp", r=None, w=None, **kw):
        r = [in_] if r is None else r
        w = [out] if w is None else w
        self._deps(q, r, w)
        i = self.dnext
        self.dnext = (self.dnext + 1) % NDMA_SEMS
        if self.dcnt[i] > 0:
            self._wait(q, (("d", i), self.dcnt[i]))
        ins = self.engs[q].dma_start(out=out, in_=in_, **kw)
        self.dcnt[i] += 16
        ins.then_inc(self.dsem[i], 16)
        self._record((("d", i), self.dcnt[i]), r, w)
        self.n_ins += 1
        return ins

    def barrier(self, engs=None):
        for e in (engs or self.engs):
            for k in self.engs:
                if self.cnt[k] > 0:
                    self._wait(e, (k, self.cnt[k]))
            for i in range(NDMA_SEMS):
                if self.dcnt[i] > 0:
                    self._wait(e, (("d", i), self.dcnt[i]))
        self.res = {}


class Pool_:
    def __init__(self, c, tag):
        self.c = c
        self.tag = tag
        self.es = ExitStack()

    def sb(self, name, shape, dt):
        self.c.uid += 1
        return self.es.enter_context(self.c.nc.sbuf_tensor("%s_%s_%d" % (self.tag, name, self.c.uid), shape, dt))

    def ps(self, name, shape, dt):
        self.c.uid += 1
        return self.es.enter_context(self.c.nc.psum_tensor("%s_%s_%d" % (self.tag, name, self.c.uid), shape, dt))

    def close(self, keep=()):
        self.c.barrier()
        self.es.close()


def host_consts():
    p = np.arange(128)
    cst = {}
    cst["c_ident"] = np.eye(128, dtype=np.float32)
    bo = np.zeros((128, 128), np.float32)
    bo[:64, :64] = 1.0
    bo[64:, 64:] = 1.0
    cst["c_bones"] = bo
    cst["c_tri"] = np.where(p[:, None] > p[None, :], NEG, 0.0).astype(np.float32)
    cst["c_low"] = np.where(p[None, :] >= p[:, None], NEG, 0.0).astype(np.float32)
    s = np.arange(T)
    cst["c_E"] = (s[None, :] // 64 == np.arange(64)[:, None]).astype(np.float32)
    t = np.arange(2560)
    cst["c_m0"] = np.where(t[None, :] < 16 * p[:, None] + 31, NEG, 0.0).astype(np.float32)
    n = np.arange(256)
    blk = np.arange(64)
    cs = n * 16
    ce = cs + 31
    ov = ((cs[:, None] < (blk[None, :] + 1) * 64) & (ce[:, None] >= blk[None, :] * 64)).astype(np.float32)
    ov[255] = 0.0
    cst["c_ov"] = ov
    tt = np.arange(T)
    cur = tt // 64
    forced = (blk[None] == 0) | (blk[None] == cur[:, None]) | (blk[None] == cur[:, None] - 1)
    valid = blk[None] * 64 <= tt[:, None]
    cst["c_bonus"] = np.where(valid, 1e4 * forced.astype(np.float32), -1e30).astype(np.float32)
    q = np.arange(64)
    cst["c_mcaus"] = np.where(q[:, None] > q[None, :], -1e30, 0.0).astype(np.float32)
    return cst


CONST_SHAPES = {"c_ident": [128, 128], "c_bones": [128, 128], "c_tri": [128, 128], "c_low": [128, 128],
                "c_E": [64, T], "c_m0": [128, 2560], "c_ov": [256, 64], "c_bonus": [T, 64],
                "c_mcaus": [64, 64]}

WEIGHT_SHAPES = {
    "ln1_g": [2, 1024], "w_in": [2, 1024, 3360], "b_in": [2, 3360], "nsa_q_norm_g": [2, 64],
    "nsa_k_norm_g": [2, 3, 64], "cmp_pe": [2, 2, 32, 64], "cmp_w1": [2, 2, 2048, 128],
    "cmp_w2": [2, 2, 128, 64], "m_conv_w": [2, 4, 1024], "m_conv_b": [2, 1024], "m_fgate_b": [2, 4],
    "nsa_out_norm_g": [2, 512], "m_out_norm_g": [2, 512], "w_out": [2, 1024, 1024], "ln2_g": [2, 1024],
    "w_gate_up": [2, 1024, 5632], "w_down": [2, 2816, 1024],
}

SCRATCH = {
    "s_qa": ([8, 64, T], BF16), "s_ks": ([2, 64, T], BF16), "s_kw": ([2, 64, T], BF16),
    "s_kc": ([128, T + 16], BF16), "s_vc": ([128, T + 16], BF16),
    "s_vs": ([T, 128], BF16), "s_vw": ([T, 128], BF16), "s_ga": ([T, 24], F32),
    "s_qb": ([4, 128, T], BF16), "s_kb": ([4, 128, T], BF16), "s_vb": ([T, 512], BF16),
    "s_ob": ([T, 512], F32), "s_if": ([T, 8], F32),
    "s_ocmp": ([T, 512], F32), "s_biasT": ([2, 64, T], BF16),
    "s_ya": ([T, 512], BF16), "s_yb": ([T, 512], BF16),
    "s_x1": ([T, 1024], F32), "s_x2": ([T, 1024], F32),
    "s_g1": ([256, 64], F32), "s_g2": ([256, 64], F32), "s_g3": ([256, 64], F32), "s_g4": ([256, 4], F32),
    "s_g5": ([4, 64], F32),
    "s_dsel": ([T, 512], F32), "s_dwin": ([T, 512], F32),
    "s_mg": ([2, 4, T], F32), "s_dec": ([4, 64], F32),
}


def rsqrt_ops(c, out, in_, scale, r, w, tmp=None):
    c.op("act", lambda e: e.activation(out=out, in_=in_, func=AF.Sqrt, scale=scale, bias=c.eps_ap[0:out.shape[0], :]), r=r + [c.eps_t], w=w)
    c.op("dve", lambda e: e.reciprocal(out=out, in_=out), r=w, w=w)


def phase_A(c, L, D, xin):
    nc = c.nc
    P = Pool_(c, "A%d" % L)
    K = c.K
    win = P.sb("win", [128, 8, DIN], BF16)
    stg = [P.sb("stg%d" % i, [128, 8, 512], F32) for i in range(2)]
    g1 = P.sb("g1", [128, 8], F32)
    c.dma(g1[:], D["ln1_g"][L].rearrange("(kc p) -> p kc", p=128), allow_slow_non_contiguous=True)
    wsrc = D["w_in"][L].rearrange("(k p) n -> p k n", p=128)
    wblocks = []
    wstate = {"next": 0}

    def load_wblock():
        i = wstate["next"]
        if i >= len(wblocks):
            return
        wstate["next"] += 1
        col, n = wblocks[i]
        s_ = stg[i % 2]
        c.dma(s_[:, :, 0:n], wsrc[:, :, col:col + n], w=[s_])
        eng = "dve" if i % 2 == 0 else "pool"
        c.op(eng, lambda e: e.tensor_tensor(out=win[:, :, col:col + n], in0=s_[:, :, 0:n], in1=g1[:].unsqueeze(2).to_broadcast([128, 8, n]), op=ALU.mult),
             r=[s_, g1], w=[("win", col)])
    fm = [("qa", O_QA + 128 * i, i) for i in range(4)] + [("kc", O_KC, 0), ("vc", O_VC, 0), ("ks", O_KS, 0), ("kw", O_KW, 0)]
    fm += [("qb", O_QB + 128 * i, i) for i in range(4)] + [("kb", O_KB + 128 * i, i) for i in range(4)]
    nfm = len(fm)
    tmg = [(O_VB, 512), (O_OB, 512), (O_VS, 128), (O_VW, 152), (O_IF, 8)]
    wblocks.extend([(col, 128) for (_, col, _) in fm] + tmg)
    bfm = P.sb("bfm", [128, nfm], F32)
    for ci, (_, col, _) in enumerate(fm):
        c.dma(bfm[:, ci:ci + 1], D["b_in"][L, col:col + 128].rearrange("(p o) -> p o", o=1), w=[bfm])
    gn = P.sb("gn", [128, 3], F32)
    for half in range(2):
        ps_ = slice(half * 64, half * 64 + 64)
        c.dma(gn[ps_, 0:1], D["nsa_q_norm_g"][L].rearrange("(p o) -> p o", o=1), w=[gn])
        c.dma(gn[ps_, 1:2], D["nsa_k_norm_g"][L, 1].rearrange("(p o) -> p o", o=1), w=[gn])
        c.dma(gn[ps_, 2:3], D["nsa_k_norm_g"][L, 2].rearrange("(p o) -> p o", o=1), w=[gn])
    c.op("dve", lambda e: e.tensor_scalar(out=gn[:, 0:1], in0=gn[:, 0:1], scalar1=0.125, scalar2=None, op0=ALU.mult), r=[gn], w=[gn])
    cw = P.sb("cw", [128, 8, 4], F32)
    cb = P.sb("cb", [128, 8], F32)
    for j in range(4):
        c.dma(cw[:, :, j], D["m_conv_w"][L, j].rearrange("(c p) -> p c", p=128), w=[cw], allow_slow_non_contiguous=True)
    c.dma(cb[:], D["m_conv_b"][L].rearrange("(c p) -> p c", p=128), allow_slow_non_contiguous=True)
    dg = P.sb("dg", [128, 8, 4, 128], BF16)
    for cc in range(8):
        for j in range(4):
            c.op("dve" if (cc + j) % 2 == 0 else "pool", lambda e: e.tensor_scalar(out=dg[:, cc, j, :], in0=K["identf"][:], scalar1=cw[:, cc, j:j + 1], scalar2=None, op0=ALU.mult),
                 r=[K["identf"], cw], w=[("dg", cc, j)])
    btm = P.sb("btm", [128, DIN], F32)
    c.dma(btm[:], D["b_in"][L].partition_broadcast(128))
    pre = P.sb("pre", [128, 8, 515], BF16)
    c.op("pool", lambda e: e.memset(pre[:], 0.0), w=[("pre", i) for i in range(8)])

    xt = [P.sb("xt%d" % i, [128, DM], F32) for i in range(2)]
    junk = P.sb("junk", [128, DM], BF16)
    ss = [P.sb("ss%d" % i, [128, 1], F32) for i in range(2)]
    hb = [P.sb("hb%d" % i, [128, DM], BF16) for i in range(2)]
    hT = [P.sb("hT%d" % i, [128, 8, 512], BF16) for i in range(2)]
    yf = [P.sb("yf%d" % i, [128, 512], F32) for i in range(2)]
    sq = [P.sb("sq%d" % i, [128, 512], BF16) for i in range(2)]
    rs = [P.sb("rs%d" % i, [128, 512], F32) for i in range(2)]
    ob16 = [P.sb("ob16_%d" % i, [128, 512], BF16) for i in range(4)]
    tv = [P.sb("tv%d" % i, [128, 512], BF16) for i in range(2)]
    to = [P.sb("to%d" % i, [128, 512], F32) for i in range(2)]
    tm = [P.sb("tm%d" % i, [128, 32], F32) for i in range(2)]
    tmb = [P.sb("tmb%d" % i, [128, 256], BF16) for i in range(2)]
    pT = [P.ps("pT%d" % i, [128, 8, 128], BF16) for i in range(2)]
    pF = [P.ps("pF%d" % i, [128, 512], F32) for i in range(2)]
    pN = P.ps("pN", [128, 512], F32)
    pV = P.ps("pV", [128, 512], F32)
    pX = [P.ps("pX%d" % i, [128, 512], F32) for i in range(2)]

    n_ob = [0]
    load_wblock()
    load_wblock()

    def stage2(g, ci, kind, idx, pF_):
        tg0 = g * 512
        o16 = ob16[n_ob[0] % 4]
        n_ob[0] += 1
        if kind in ("qa", "ks", "kw"):
            yf_, sq_, rs_ = yf[ci % 2], sq[ci % 2], rs[ci % 2]
            c.op("pe", lambda e: e.matmul(pN[:], lhsT=K["bonesb"][:], rhs=sq_[:], start=True, stop=True), r=[sq_, K["bonesb"]], w=[pN])
            rsqrt_ops(c, rs_[:], pN[:], 1.0 / 64, [pN], [rs_])
            gcol = {"qa": 0, "ks": 1, "kw": 2}[kind]
            c.op("dve", lambda e: e.scalar_tensor_tensor(out=o16[:], in0=yf_[:], scalar=gn[:, gcol:gcol + 1], in1=rs_[:], op0=ALU.mult, op1=ALU.mult),
                 r=[yf_, gn, rs_], w=[o16])
            if kind == "qa":
                dst = D["s_qa"][2 * idx:2 * idx + 2, :, tg0:tg0 + 512].rearrange("h p t -> (h p) t")
                wk = [("s_qa", 2 * idx, g), ("s_qa", 2 * idx + 1, g)]
            else:
                nm = "s_" + kind
                dst = D[nm][:, :, tg0:tg0 + 512].rearrange("h p t -> (h p) t")
                wk = [(nm, 0, g), (nm, 1, g)]
            c.dma(dst, o16[:], q="sp", w=wk)
        else:
            cc = idx + (0 if kind == "qb" else 4)
            pk = ("pre", cc)
            for j in range(4):
                c.op("pe", lambda e: e.matmul(pV[:], lhsT=dg[:, cc, j, :], rhs=pre[:, cc, j:j + 512], start=(j == 0), stop=(j == 3)), r=[pk, ("dg", cc, j)], w=[pV])
            c.op("pool", lambda e: e.tensor_copy(out=pre[:, cc, 0:3], in_=pre[:, cc, 512:515]), r=[pk], w=[pk])
            c.op("act", lambda e: e.activation(out=o16[:], in_=pV[:], func=AF.Silu, bias=cb[:, cc:cc + 1]), r=[pV, cb], w=[o16])
            nm = "s_" + kind
            c.dma(D[nm][idx, :, tg0:tg0 + 512], o16[:], q="sp", w=[(nm, idx, g)])

    for g in range(NG):
        hTg = hT[g % 2]
        for ti in range(4):
            it = g * 4 + ti
            x_ = xt[it % 2]
            ss_ = ss[it % 2]
            hb_ = hb[it % 2]
            pT_ = pT[it % 2]
            t0 = it * 128
            c.dma(x_[:], xin[t0:t0 + 128, :], r=[("x", it)])
            c.op("act", lambda e: e.activation(out=junk[:], in_=x_[:], func=AF.Square, accum_out=ss_[:]), r=[x_], w=[junk, ss_])
            rsqrt_ops(c, ss_[:], ss_[:], 1.0 / DM, [ss_], [ss_])
            c.op("dve", lambda e: e.tensor_scalar(out=hb_[:], in0=x_[:], scalar1=ss_[:, 0:1], scalar2=None, op0=ALU.mult), r=[x_, ss_], w=[hb_])
            for k in range(8):
                c.op("pe", lambda e: e.transpose(out=pT_[:, k, :], in_=hb_[:, k * 128:(k + 1) * 128], identity=K["identb"][:]), r=[hb_, K["identb"]], w=[pT_])
            c.op("act", lambda e: e.copy(out=hTg[:, :, ti * 128:(ti + 1) * 128], in_=pT_[:]), r=[pT_], w=[hTg])
        tg0 = g * 512
        pending = None
        for ci, (kind, col, idx) in enumerate(fm):
            pF_ = pF[ci % 2]
            if g == 0:
                load_wblock()
            for k in range(8):
                c.op("pe", lambda e: e.matmul(pF_[:], lhsT=win[:, k, col:col + 128], rhs=hTg[:, k, :], start=(k == 0), stop=(k == 7)),
                     r=[hTg, ("win", col)], w=[pF_])
            bias = bfm[:, ci:ci + 1]
            if kind in ("qa", "ks", "kw"):
                yf_, sq_ = yf[ci % 2], sq[ci % 2]
                c.op("act", lambda e: e.activation(out=yf_[:], in_=pF_[:], func=AF.Identity, bias=bias), r=[pF_, bfm], w=[yf_])
                c.op("act", lambda e: e.activation(out=sq_[:], in_=pF_[:], func=AF.Square, bias=bias), r=[pF_, bfm], w=[sq_])
            elif kind in ("kc", "vc"):
                o16 = ob16[n_ob[0] % 4]
                n_ob[0] += 1
                c.op("act", lambda e: e.activation(out=o16[:], in_=pF_[:], func=AF.Identity, bias=bias), r=[pF_, bfm], w=[o16])
                nm = "s_" + kind
                c.dma(D[nm][:, tg0:tg0 + 512], o16[:], q="sp", w=[(nm, g)])
            else:
                cc = idx + (0 if kind == "qb" else 4)
                c.op("act", lambda e: e.activation(out=pre[:, cc, 3:515], in_=pF_[:], func=AF.Identity, bias=bias), r=[pF_, bfm], w=[("pre", cc)])
            if pending is not None:
                stage2(*pending)
            pending = (g, ci, kind, idx, pF_) if kind not in ("kc", "vc") else None
        if g == 0:
            while wstate["next"] < len(wblocks):
                load_wblock()
        for ti in range(4):
            it = g * 4 + ti
            t0 = it * 128
            lt = lambda k: hTg[:, k, ti * 128:(ti + 1) * 128]

            def proj(pp, po, col, n):
                for k in range(8):
                    c.op("pe", lambda e: e.matmul(pp[:, po:po + n], lhsT=lt(k), rhs=win[:, k, col:col + n], start=(k == 0), stop=(k == 7)),
                         r=[hTg, ("win", col)], w=[(pp.name, po)])

            tv_, to_, tm_, tmb_ = tv[it % 2], to[it % 2], tm[it % 2], tmb[it % 2]
            proj(pX[0], 0, O_VB, 512)
            proj(pX[1], 0, O_OB, 512)
            if ti == 0 and pending is not None:
                stage2(*pending)
                pending = None
            c.op("dve", lambda e: e.tensor_tensor(out=tv_[:], in0=pX[0][:], in1=btm[:, O_VB:O_VB + 512], op=ALU.add), r=[(pX[0].name, 0), btm], w=[tv_])
            c.dma(D["s_vb"][t0:t0 + 128, :], tv_[:], q="sp", w=[("s_vb", it)])
            c.op("dve", lambda e: e.tensor_tensor(out=to_[:], in0=pX[1][:], in1=btm[:, O_OB:O_OB + 512], op=ALU.add), r=[(pX[1].name, 0), btm], w=[to_])
            c.op("act", lambda e: e.activation(out=to_[:], in_=to_[:], func=AF.Sigmoid), r=[to_], w=[to_])
            c.dma(D["s_ob"][t0:t0 + 128, :], to_[:], q="sp", w=[("s_ob", it)])
            pck = [(pX[0].name, 0), (pX[0].name, 128), (pX[0].name, 280)]
            for (po, col, n) in ((0, O_VS, 128), (128, O_VW, 152), (280, O_IF, 8)):
                for k in range(8):
                    c.op("pe", lambda e: e.matmul(pX[0][:, po:po + n], lhsT=lt(k), rhs=win[:, k, col:col + n], start=(k == 0), stop=(k == 7)),
                         r=[hTg, ("win", col)], w=pck)
            c.op("dve", lambda e: e.tensor_tensor(out=tmb_[:, 0:128], in0=pX[0][:, 0:128], in1=btm[:, O_VS:O_VS + 128], op=ALU.add), r=pck + [btm], w=[tmb_])
            c.op("dve", lambda e: e.tensor_tensor(out=tmb_[:, 128:256], in0=pX[0][:, 128:256], in1=btm[:, O_VW:O_VW + 128], op=ALU.add), r=pck + [btm, tmb_], w=[tmb_])
            c.op("dve", lambda e: e.tensor_tensor(out=tm_[:, 0:24], in0=pX[0][:, 256:280], in1=btm[:, O_GA:O_GA + 24], op=ALU.add), r=pck + [btm], w=[tm_])
            c.op("dve", lambda e: e.tensor_tensor(out=tm_[:, 24:32], in0=pX[0][:, 280:288], in1=btm[:, O_IF:O_IF + 8], op=ALU.add), r=pck + [btm, tm_], w=[tm_])
            c.op("act", lambda e: e.activation(out=tm_[:, 0:24], in_=tm_[:, 0:24], func=AF.Sigmoid), r=[tm_], w=[tm_])
            c.dma(D["s_vs"][t0:t0 + 128, :], tmb_[:, 0:128], q="sp", w=[("s_vs", it)])
            c.dma(D["s_vw"][t0:t0 + 128, :], tmb_[:, 128:256], q="sp", w=[("s_vw", it)])
            c.dma(D["s_ga"][t0:t0 + 128, :], tm_[:, 0:24], q="sp", w=[("s_ga", it)])
            c.dma(D["s_if"][t0:t0 + 128, :], tm_[:, 24:32], q="sp", w=[("s_if", it)])
    P.close()


def load_weight_bf16(c, P, dst, src_rows, nk, ncols, stg, scale=None, key="w"):
    for k in range(nk):
        s = stg[k % len(stg)]
        c.dma(s[:, 0:ncols], src_rows(k))
        eng = "dve" if k % 2 == 0 else "pool"
        if scale is None:
            c.op(eng, lambda e: e.tensor_copy(out=dst[:, k, :], in_=s[:, 0:ncols]), r=[s], w=[(key, k)])
        else:
            c.op(eng, lambda e: e.tensor_scalar(out=dst[:, k, :], in0=s[:, 0:ncols], scalar1=scale[:, k:k + 1], scalar2=None, op0=ALU.mult),
                 r=[s, scale], w=[(key, k)])


def phase_D(c, L, D, xin, EW):
    K = c.K
    P = Pool_(c, "D%d" % L)
    wo = P.sb("wo", [128, 8, DM], BF16)
    stg = [P.sb("stg%d" % i, [128, DM], F32) for i in range(2)]
    load_weight_bf16(c, P, wo, lambda k: D["w_out"][L, k * 128:(k + 1) * 128, :], 8, DM, stg, key="wo")
    xt = [P.sb("xt%d" % i, [128, DM], F32) for i in range(2)]
    yc = [P.sb("yc%d" % i, [128, DM], BF16) for i in range(2)]
    yT = [P.sb("yT%d" % i, [128, 8, 128], BF16) for i in range(2)]
    pT = [P.ps("pT%d" % i, [128, 8, 128], BF16) for i in range(2)]
    pO = [P.ps("pO%d" % i, [128, 2, 512], F32) for i in range(2)]
    for it in range(32):
        t0 = it * 128
        x_, yc_, yT_, pT_, pO_ = xt[it % 2], yc[it % 2], yT[it % 2], pT[it % 2], pO[it % 2]
        c.dma(x_[:], xin[t0:t0 + 128, :], r=[("x", it)])
        c.dma(yc_[:, 0:512], D["s_ya"][t0:t0 + 128, :], r=[("s_ya", it)], w=[yc_])
        c.dma(yc_[:, 512:1024], D["s_yb"][t0:t0 + 128, :], r=[("s_yb", it)], w=[yc_])
        for k in range(8):
            c.op("pe", lambda e: e.transpose(out=pT_[:, k, :], in_=yc_[:, k * 128:(k + 1) * 128], identity=K["identb"][:]), r=[yc_, K["identb"]], w=[pT_])
        c.op("act", lambda e: e.copy(out=yT_[:], in_=pT_[:]), r=[pT_], w=[yT_])
        for hf in range(2):
            for k in range(8):
                c.op("pe", lambda e: e.matmul(pO_[:, hf, :], lhsT=yT_[:, k, :], rhs=wo[:, k, hf * 512:(hf + 1) * 512], start=(k == 0), stop=(k == 7)),
                     r=[yT_, ("wo", k)], w=[pO_])
        c.op("dve", lambda e: e.tensor_tensor(out=x_[:], in0=x_[:], in1=pO_[:].rearrange("p a b -> p (a b)"), op=ALU.add), r=[x_, pO_], w=[x_])
        c.dma(D["s_x1"][t0:t0 + 128, :], x_[:], q="sp", w=[("x1", it)])
        if EW is not None:
            EW["load_gu"]()
            EW["load_wd"]()
    P.close(keep=[k for k in c.res if isinstance(k, tuple) and k[0] in ("wgu", "wd", "x1")] + ([EW["g2"].name] if EW else []))


def phase_E_weights(c, L, D):
    P = Pool_(c, "EW%d" % L)
    wgu = P.sb("wgu", [128, 22, 8, 256], BF16)
    wd = P.sb("wd", [128, 22, DM], BF16)
    stg = [P.sb("stg%d" % i, [128, 8, 256], F32) for i in range(2)]
    g2 = P.sb("g2", [128, 8], F32)
    c.dma(g2[:], D["ln2_g"][L].rearrange("(kc p) -> p kc", p=128), allow_slow_non_contiguous=True)
    gsrc = D["w_gate_up"][L].rearrange("(k p) n -> p k n", p=128)
    wst = {"n": 0, "gu": 0, "wd": 0}

    def load_gu():
        fc = wst["gu"]
        if fc >= 22:
            return
        wst["gu"] += 1
        s_ = stg[wst["n"] % 2]
        eng = "dve" if wst["n"] % 2 == 0 else "pool"
        wst["n"] += 1
        c.dma(s_[:, :, 0:128], gsrc[:, :, fc * 128:(fc + 1) * 128], w=[(s_.name, 0)])
        c.dma(s_[:, :, 128:256], gsrc[:, :, FF + fc * 128:FF + (fc + 1) * 128], w=[(s_.name, 1)])
        c.op(eng, lambda e: e.tensor_tensor(out=wgu[:, fc, :, :], in0=s_[:], in1=g2[:].unsqueeze(2).to_broadcast([128, 8, 256]), op=ALU.mult),
             r=[(s_.name, 0), (s_.name, 1), g2], w=[("wgu", fc)])

    def load_wd():
        k = wst["wd"]
        if k >= 22:
            return
        wst["wd"] += 1
        s_ = stg[wst["n"] % 2]
        eng = "dve" if wst["n"] % 2 == 0 else "pool"
        wst["n"] += 1
        sv = s_[:].rearrange("p a b -> p (a b)")[:, 0:DM]
        c.dma(sv, D["w_down"][L, k * 128:(k + 1) * 128, :], w=[(s_.name, 0), (s_.name, 1)])
        c.op(eng, lambda e: e.tensor_copy(out=wd[:, k, :], in_=sv), r=[(s_.name, 0), (s_.name, 1)], w=[("wd", k)])

    return {"P": P, "wgu": wgu, "wd": wd, "g2": g2, "load_gu": load_gu, "load_wd": load_wd}


def phase_E(c, L, D, xout, EW):
    K = c.K
    P = Pool_(c, "E%d" % L)
    wgu, wd, load_gu, load_wd = EW["wgu"], EW["wd"], EW["load_gu"], EW["load_wd"]
    xt = P.sb("xt", [128, DM], F32)
    xo = P.sb("xo", [128, DM], F32)
    junk = P.sb("junk", [128, DM], BF16)
    ss = [P.sb("ss%d" % i, [128, 1], F32) for i in range(2)]
    hb = [P.sb("hb%d" % i, [128, DM], BF16) for i in range(2)]
    hT = P.sb("hT", [128, 8, 512], BF16)
    sl = [P.sb("sl%d" % i, [128, 512], BF16) for i in range(2)]
    aT = P.sb("aT", [128, 22, 512], BF16)
    pT = [P.ps("pT%d" % i, [128, 8, 128], BF16) for i in range(2)]
    pO = P.ps("pO", [128, 2, 512], F32)
    pG = [P.ps("pG%d" % i, [128, 512], F32) for i in range(2)]
    pU = [P.ps("pU%d" % i, [128, 512], F32) for i in range(2)]
    for g in range(NG):
        for ti in range(4):
            it = g * 4 + ti
            t0 = it * 128
            pT_ = pT[it % 2]
            c.dma(xt[:], D["s_x1"][t0:t0 + 128, :], r=[("x1", it)])
            ss_, hb_ = ss[it % 2], hb[it % 2]
            c.op("act", lambda e: e.activation(out=junk[:], in_=xt[:], func=AF.Square, accum_out=ss_[:]), r=[xt], w=[junk, ss_])
            rsqrt_ops(c, ss_[:], ss_[:], 1.0 / DM, [ss_], [ss_])
            c.op("dve", lambda e: e.tensor_scalar(out=hb_[:], in0=xt[:], scalar1=ss_[:, 0:1], scalar2=None, op0=ALU.mult), r=[xt, ss_], w=[hb_])
            for k in range(8):
                c.op("pe", lambda e: e.transpose(out=pT_[:, k, :], in_=hb_[:, k * 128:(k + 1) * 128], identity=K["identb"][:]), r=[hb_, K["identb"]], w=[pT_])
            c.op("act", lambda e: e.copy(out=hT[:, :, ti * 128:(ti + 1) * 128], in_=pT_[:]), r=[pT_], w=[hT])
        for fc in range(22):
            pG_, pU_, sl_ = pG[fc % 2], pU[fc % 2], sl[fc % 2]
            if g == 0:
                load_gu()
                load_wd()
            for k in range(8):
                c.op("pe", lambda e: e.matmul(pG_[:], lhsT=wgu[:, fc, k, 0:128], rhs=hT[:, k, :], start=(k == 0), stop=(k == 7)),
                     r=[hT, ("wgu", fc)], w=[pG_])
            for k in range(8):
                c.op("pe", lambda e: e.matmul(pU_[:], lhsT=wgu[:, fc, k, 128:256], rhs=hT[:, k, :], start=(k == 0), stop=(k == 7)),
                     r=[hT, ("wgu", fc)], w=[pU_])
            c.op("act", lambda e: e.activation(out=sl_[:], in_=pG_[:], func=AF.Silu), r=[pG_], w=[sl_])
            c.op("dve", lambda e: e.tensor_tensor(out=aT[:, fc, :], in0=sl_[:], in1=pU_[:], op=ALU.mult), r=[sl_, pU_], w=[("aT", fc)])
        for ti in range(4):
            it = g * 4 + ti
            t0 = it * 128
            for hf in range(2):
                for fc in range(22):
                    c.op("pe", lambda e: e.matmul(pO[:, hf, :], lhsT=aT[:, fc, ti * 128:(ti + 1) * 128], rhs=wd[:, fc, hf * 512:(hf + 1) * 512], start=(fc == 0), stop=(fc == 21)),
                         r=[("aT", fc), ("wd", fc)], w=[("pO", hf)])
            c.dma(xo[:], D["s_x1"][t0:t0 + 128, :], r=[("x1", it)])
            c.op("dve", lambda e: e.tensor_tensor(out=xo[:], in0=xo[:], in1=pO[:].rearrange("p a b -> p (a b)"), op=ALU.add),
                 r=[xo, ("pO", 0), ("pO", 1)], w=[xo])
            c.dma(xout[t0:t0 + 128, :], xo[:], q="sp", w=[("x", it)])
    P.close()
    EW["P"].es.close()


def build_program(phases=("A", "B", "C", "D", "E"), depth=DEPTH, debug_out=(), ext_in=()):
    nc = bass.Bass("TRN2", target_bir_lowering=False)
    D = {}
    D["x"] = nc.dram_tensor("x", [T, DM], F32, kind="ExternalInput").ap()
    for k, shp in WEIGHT_SHAPES.items():
        D[k] = nc.dram_tensor(k, shp, F32, kind="ExternalInput").ap()
    for k, shp in CONST_SHAPES.items():
        D[k] = nc.dram_tensor(k, shp, F32, kind="ExternalInput").ap()
    D["out"] = nc.dram_tensor("out", [T, DM], F32, kind="ExternalOutput").ap()
    for k, (shp, dt) in SCRATCH.items():
        kind = "ExternalOutput" if k in debug_out else ("ExternalInput" if k in ext_in else "Internal")
        D[k] = nc.dram_tensor(k, shp, dt, kind=kind).ap()
    with ExitStack() as es:
        c = Ctx(nc, es)
        c.debug = len(debug_out) > 0
        KP = Pool_(c, "K")
        K = {}
        c.K = K
        c.eps_t = KP.sb("eps", [128, 1], F32)
        c.eps_ap = c.eps_t[:]
        c.op("pool", lambda e: e.memset(c.eps_t[:], EPS), w=[c.eps_t])
        c.one_t = KP.sb("one", [128, 1], F32)
        c.op("pool", lambda e: e.memset(c.one_t[:], 1.0), w=[c.one_t])
        cst = KP.sb("cstage", [128, 128], F32)
        for nm, src in (("identb", "c_ident"), ("bonesb", "c_bones"), ("trib", "c_tri"), ("lowb", "c_low")):
            K[nm] = KP.sb(nm, [128, 128], BF16)
            c.dma(cst[:], D[src])
            c.op("dve", lambda e: e.tensor_copy(out=K[nm][:], in_=cst[:]), r=[cst], w=[K[nm]])
        K["identf"] = KP.sb("identf", [128, 128], F32)
        c.dma(K["identf"][:], D["c_ident"])
        xin = D["x"]
        for L in range(depth):
            last = (L == depth - 1)
            xout = D["out"] if last else D["s_x2"]
            if "A" in phases:
                phase_A(c, L, D, xin)
            if "B" in phases:
                phase_B(c, L, D)
            if "C" in phases:
                phase_C(c, L, D)
            EW = phase_E_weights(c, L, D) if "E" in phases else None
            if "D" in phases:
                phase_D(c, L, D, xin, EW)
            if "E" in phases:
                phase_E(c, L, D, xout, EW)
            xin = xout
        c.barrier(["sp"])
        print("instructions", c.n_ins, "waits", c.n_wait)
    return nc


def phase_B(c, L, D):
    K = c.K
    P = Pool_(c, "B%d" % L)
    identb, trib, lowb = K["identb"], K["trib"], K["lowb"]
    KA = [P.sb("KA%d" % g, [128, T], BF16) for g in range(2)]
    KW = P.sb("KW", [64, 2, T], BF16)
    VSa = P.sb("VSa", [128, 2, 32, 65], BF16)
    VWa = P.sb("VWa", [128, 2, 32, 65], BF16)
    KcT = P.sb("KcT", [64, 2, 256], BF16)
    Ra = P.sb("Ra", [128, 2, 2, 129], BF16)
    M0b = P.sb("M0b", [128, 2560], BF16)
    gout = P.sb("gout", [128, 512], F32)
    c.dma(gout[:], D["nsa_out_norm_g"][L].partition_broadcast(128))
    P1 = Pool_(c, "B1_%d" % L)
    estg = P1.sb("estg", [128, T], F32)
    c.dma(estg[64:128, :], D["c_E"])
    for g in range(2):
        c.op("dve" if g == 0 else "pool", lambda e: e.tensor_copy(out=KA[g][64:128, :], in_=estg[64:128, :]), r=[estg], w=[KA[g]])
        c.dma(KA[g][0:64, :], D["s_ks"][g], r=[("s_ks", g, i) for i in range(NG)], w=[KA[g]])
        c.dma(KW[:, g, :], D["s_kw"][g], r=[("s_kw", g, i) for i in range(NG)], w=[KW])
        c.dma(VSa[:, g, :, 0:64], D["s_vs"][:, g * 64:(g + 1) * 64].rearrange("(kt p) d -> p kt d", p=128), r=[("s_vs", i) for i in range(32)], w=[VSa])
        c.dma(VWa[:, g, :, 0:64], D["s_vw"][:, g * 64:(g + 1) * 64].rearrange("(kt p) d -> p kt d", p=128), r=[("s_vw", i) for i in range(32)], w=[VWa])
    c.op("pool", lambda e: e.memset(VSa[:, :, :, 64:65], 1.0), r=[VSa], w=[VSa])
    c.op("pool", lambda e: e.memset(VWa[:, :, :, 64:65], 1.0), r=[VWa], w=[VWa])
    m0s = P1.sb("m0s", [128, 2560], F32)
    c.dma(m0s[:], D["c_m0"])
    c.op("dve", lambda e: e.tensor_copy(out=M0b[:], in_=m0s[:]), r=[m0s], w=[M0b])
    ovs = P1.sb("ovs", [128, 2, 64], F32)
    c.dma(ovs[:], D["c_ov"].rearrange("(c p) j -> p c j", p=128))
    c.op("pool", lambda e: e.memset(Ra[:], 0.0), w=[Ra])
    for g in range(2):
        c.op("dve", lambda e: e.tensor_copy(out=Ra[:, g, :, 65:129], in_=ovs[:]), r=[ovs, Ra], w=[Ra])
        c.op("pool", lambda e: e.memset(Ra[:, g, :, 64:65], 1.0), r=[Ra], w=[Ra])
    kg0 = P1.sb("kg0", [128, 64], F32)
    c.dma(kg0[:], D["nsa_k_norm_g"][L, 0].partition_broadcast(128))
    X2 = [P1.sb("X2_%d" % i, [128, T], BF16) for i in range(2)]
    w1s = P1.sb("w1s", [128, 16, 128], F32)
    w1b = [P1.sb("w1b%d" % i, [128, 16, 128], BF16) for i in range(2)]
    pes = P1.sb("pes", [128, 16], F32)
    peb = [P1.sb("peb%d" % i, [128, 16], BF16) for i in range(2)]
    w2s = P1.sb("w2s", [128, 64], F32)
    w2b = [P1.sb("w2b%d" % i, [128, 64], BF16) for i in range(2)]
    hbias = [P1.sb("hbias%d" % i, [128, 1], F32) for i in range(2)]
    hid = [P1.sb("hid%d" % i, [128, 256], BF16) for i in range(2)]
    kcf = P1.sb("kcf", [128, 64], F32)
    kjunk = P1.sb("kjunk", [128, 64], F32)
    kss = P1.sb("kss", [128, 1], F32)
    kcb = P1.sb("kcb", [128, 64], BF16)
    pH = P1.ps("pH", [128, 256], F32)
    pHb = P1.ps("pHb", [128, 1], F32)
    pK = P1.ps("pK", [128, 64], F32)
    pKT = P1.ps("pKT", [64, 128], BF16)
    for wi in range(2):
        c.dma(w1s[:], D["cmp_w1"][L, wi].rearrange("(l p) h -> p l h", p=128))
        c.op("dve", lambda e: e.tensor_copy(out=w1b[wi][:], in_=w1s[:]), r=[w1s], w=[w1b[wi]])
        c.dma(pes[:], D["cmp_pe"][L, wi].rearrange("l d -> (l d)").rearrange("(l p) -> p l", p=128), allow_slow_non_contiguous=True)
        c.op("dve", lambda e: e.tensor_copy(out=peb[wi][:], in_=pes[:]), r=[pes], w=[peb[wi]])
        c.dma(w2s[:], D["cmp_w2"][L, wi])
        c.op("dve", lambda e: e.tensor_copy(out=w2b[wi][:], in_=w2s[:]), r=[w2s], w=[w2b[wi]])
        for l in range(16):
            c.op("pe", lambda e: e.matmul(pHb[:], lhsT=w1b[wi][:, l, :], rhs=peb[wi][:, l:l + 1], start=(l == 0), stop=(l == 15)), r=[w1b[wi], peb[wi]], w=[pHb])
        c.op("act", lambda e: e.copy(out=hbias[wi][:], in_=pHb[:]), r=[pHb], w=[hbias[wi]])
        src = D["s_kc"] if wi == 0 else D["s_vc"]
        sk = "s_kc" if wi == 0 else "s_vc"
        for g in range(2):
            X = X2[g]
            c.dma(X[0:64, :], src[g * 64:(g + 1) * 64, 0:T], r=[(sk, i) for i in range(NG)], w=[X])
            c.dma(X[64:128, 0:T - 1], src[g * 64:(g + 1) * 64, 1:T], r=[(sk, i) for i in range(NG)], w=[X])
            hd = hid[g]
            for l in range(16):
                c.op("pe", lambda e: e.matmul(pH[:, 0:255], lhsT=w1b[wi][:, l, :], rhs=X[:, 2 * l:2 * l + 16 * 254 + 1:16], start=(l == 0), stop=(l == 15)),
                     r=[w1b[wi], X], w=[pH])
            c.op("pool", lambda e: e.memset(hd[:, 255:256], 0.0), w=[hd])
            c.op("act", lambda e: e.activation(out=hd[:, 0:255], in_=pH[:, 0:255], func=AF.Silu, bias=hbias[wi][:, 0:1]), r=[pH, hbias[wi], hd], w=[hd])
            for cch in range(2):
                rows = 128 if cch == 0 else 127
                c.op("pe", lambda e: e.matmul(pK[0:rows, :], lhsT=hd[:, cch * 128:cch * 128 + rows], rhs=w2b[wi][:], start=True, stop=True), r=[hd, w2b[wi]], w=[pK])
                if wi == 0:
                    c.op("act", lambda e: e.activation(out=kjunk[0:rows, :], in_=pK[0:rows, :], func=AF.Square, accum_out=kss[0:rows, :]), r=[pK], w=[kjunk, kss])
                    rsqrt_ops(c, kss[0:rows, :], kss[0:rows, :], 1.0 / 64, [kss], [kss])
                    c.op("dve", lambda e: e.scalar_tensor_tensor(out=kcb[0:rows, :], in0=pK[0:rows, :], scalar=kss[0:rows, 0:1], in1=kg0[0:rows, :], op0=ALU.mult, op1=ALU.mult),
                         r=[pK, kss, kg0], w=[kcb])
                    c.op("pe", lambda e: e.transpose(out=pKT[:, 0:rows], in_=kcb[0:rows, :], identity=identb[0:rows, 0:rows]), r=[kcb, identb], w=[pKT])
                    c.op("act", lambda e: e.copy(out=KcT[:, g, cch * 128:cch * 128 + rows], in_=pKT[:, 0:rows]), r=[pKT], w=[KcT])
                else:
                    c.op("act", lambda e: e.copy(out=Ra[0:rows, g, cch, 0:64], in_=pK[0:rows, :]), r=[pK, Ra], w=[Ra])
    P1.close()

    QA = [P.sb("QA%d" % i, [128, 8, 512], BF16) for i in range(2)]
    oacc = [P.sb("oacc%d" % i, [128, 4, 8, 64], F32) for i in range(2)]
    gat = [P.sb("gat%d" % i, [128, 4, 24], F32) for i in range(2)]
    bon = [P.sb("bon%d" % i, [128, 4, 64], F32) for i in range(2)]
    PT = [P.sb("PT%d" % i, [128, 512], BF16) for i in range(4)]
    impa = P.sb("impa", [128, 4, 64], F32)
    imt = P.sb("imt", [128, 4, 64], F32)
    rz = [P.sb("rz%d" % i, [128, 4], F32) for i in range(2)]
    gz = [P.sb("gz%d" % i, [128, 4], F32) for i in range(2)]
    otmp = [P.sb("otmp%d" % i, [128, 4, 64], F32) for i in range(2)]
    m8 = P.sb("m8", [128, 16], F32)
    sc2 = P.sb("sc2", [128, 64], F32)
    b16 = P.sb("b16", [128, 4, 64], BF16)
    bT = [P.sb("bT%d" % i, [64, 512], BF16) for i in range(2)]
    osq = P.sb("osq", [128, 4, 8, 64], F32)
    ossq = P.sb("ossq", [128, 32], F32)
    yab = [P.sb("yab%d" % i, [128, 4, 512], BF16) for i in range(2)]
    pS = [P.ps("pS%d" % i, [128, 512], F32) for i in range(3)]
    pOc = P.ps("pOc", [128, 4, 256], F32)
    pOs = P.ps("pOs", [128, 4, 65], F32)
    pOw = P.ps("pOw", [128, 4, 65], F32)
    pTk = P.ps("pTk", [64, 512], BF16)
    st = {"ns": 0, "npt": 0}

    def score_tile():
        st["ns"] += 1
        return pS[st["ns"] % 3]

    def pt_tile():
        st["npt"] += 1
        return PT[st["npt"] % 4]

    LA = 2
    stq = []

    def pop_tile():
        pS_, exp_fn, pv_fn, after = stq.pop(0)
        PT_ = pt_tile()
        exp_fn(pS_, PT_)
        pv_fn(PT_)
        if after is not None:
            after()

    def push_tile(score_fn, exp_fn, pv_fn, after=None):
        pS_ = score_tile()
        score_fn(pS_)
        stq.append((pS_, exp_fn, pv_fn, after))
        while len(stq) > LA:
            pop_tile()

    def B2(qg):
        par = qg % 2
        tg0 = qg * 512
        QA_, oacc_, gat_, bon_ = QA[par], oacc[par], gat[par], bon[par]
        c.dma(QA_[0:64, :, :], D["s_qa"][:, :, tg0:tg0 + 512].rearrange("h p t -> p h t"), r=[("s_qa", h, qg) for h in range(8)], w=[("QAq", par)])
        c.dma(gat_[:], D["s_ga"][tg0:tg0 + 512, :].rearrange("(s p) c -> p s c", p=128), r=[("s_ga", qg * 4 + i) for i in range(4)], w=[gat_])
        c.dma(bon_[:], D["c_bonus"][tg0:tg0 + 512, :].rearrange("(s p) j -> p s j", p=128), w=[bon_])

        def post_head(g, hh):
            h = g * 4 + hh
            rz_, gz_ = rz[h % 2], gz[h % 2]
            c.op("dve", lambda e: e.tensor_scalar(out=rz_[:], in0=pOc[:, :, 64], scalar1=1e-30, scalar2=None, op0=ALU.max), r=[pOc], w=[rz_])
            c.op("dve", lambda e: e.reciprocal(out=rz_[:], in_=rz_[:]), r=[rz_], w=[rz_])
            c.op("dve", lambda e: e.tensor_tensor(out=gz_[:], in0=rz_[:], in1=gat_[:, :, 3 * h], op=ALU.mult), r=[rz_, gat_], w=[gz_])
            tgt = impa if hh == 0 else imt
            c.op("dve", lambda e: e.tensor_tensor(out=tgt[:], in0=pOc[:, :, 65:129], in1=rz_[:].unsqueeze(2).to_broadcast([128, 4, 64]), op=ALU.mult),
                 r=[pOc, rz_], w=[tgt])
            if hh > 0:
                c.op("pool", lambda e: e.tensor_tensor(out=impa[:], in0=impa[:], in1=imt[:], op=ALU.add), r=[impa, imt], w=[impa])
            c.op("dve", lambda e: e.tensor_tensor(out=oacc_[:, :, h, :], in0=pOc[:, :, 0:64], in1=gz_[:].unsqueeze(2).to_broadcast([128, 4, 64]), op=ALU.mult),
                 r=[pOc, gz_], w=[("oacc", par, h)])
            if c.debug:
                c.dma(D["s_ocmp"][tg0:tg0 + 512, h * 64:(h + 1) * 64].rearrange("(s p) d -> p s d", p=128), oacc_[:, :, h, :], r=[("oacc", par, h)], w=[("dbg_ocmp", qg, h)])
            if hh == 3:
                topk(g)

        def topk(g):
            c.op("dve", lambda e: e.tensor_tensor(out=impa[:], in0=impa[:], in1=bon_[:], op=ALU.add), r=[impa, bon_], w=[impa])
            for sub in range(4):
                c.op("dve", lambda e: e.max(out=m8[:, 0:8], in_=impa[:, sub, :]), r=[impa], w=[m8])
                c.op("dve", lambda e: e.match_replace(out=sc2[:], in_to_replace=m8[:, 0:8], in_values=impa[:, sub, :], imm_value=-3e38), r=[impa, m8], w=[sc2])
                c.op("dve", lambda e: e.max(out=m8[:, 8:16], in_=sc2[:]), r=[sc2, m8], w=[m8])
                c.op("dve", lambda e: e.tensor_scalar(out=b16[:, sub, :], in0=impa[:, sub, :], scalar1=m8[:, 15:16], scalar2=NEG, op0=ALU.is_lt, op1=ALU.mult),
                     r=[impa, m8], w=[b16])
            for sub in range(4):
                c.op("pe", lambda e: e.transpose(out=pTk[:, sub * 128:(sub + 1) * 128], in_=b16[:, sub, :], identity=identb[:]), r=[b16, identb], w=[pTk])
            bT_ = bT[g]
            c.op("act", lambda e: e.copy(out=bT_[:], in_=pTk[:]), r=[pTk], w=[bT_])
            for hh in range(4):
                c.dma(QA_[64:128, g * 4 + hh, :], bT_[:], r=[bT_], w=[("QAb", par, g * 4 + hh)])
            if c.debug:
                c.dma(D["s_biasT"][g, :, tg0:tg0 + 512], bT_[:], r=[bT_], w=[("dbg_bT", qg, g)])

        for g in range(2):
            for hh in range(4):
                h = g * 4 + hh
                chunks = [0] if qg < 4 else [0, 1]
                for ci_, cch in enumerate(chunks):
                    rows = 128 if cch == 0 else 127
                    masked = (cch == 0 and qg <= 4) or (cch == 1)

                    def score(pS_, g=g, h=h, cch=cch, rows=rows, masked=masked):
                        c.op("pe", lambda e: e.matmul(pS_[0:rows, :], lhsT=KcT[:, g, cch * 128:cch * 128 + rows], rhs=QA_[0:64, h, :], start=True, stop=not masked),
                             r=[KcT, ("QAq", par)], w=[pS_])
                        if masked:
                            mc = 512 * qg if cch == 0 else 512 * (qg - 4)
                            c.op("pe", lambda e: e.matmul(pS_[0:rows, :], lhsT=identb[0:rows, 0:rows], rhs=M0b[0:rows, mc:mc + 512], start=False, stop=True),
                                 r=[identb, M0b], w=[pS_])

                    def expf(pS_, PT_, rows=rows):
                        c.op("act", lambda e: e.activation(out=PT_[0:rows, :], in_=pS_[0:rows, :], func=AF.Exp), r=[pS_], w=[PT_])

                    def pv(PT_, g=g, cch=cch, rows=rows, ci_=ci_, nch=len(chunks)):
                        for sub in range(4):
                            c.op("pe", lambda e: e.matmul(pOc[:, sub, 0:129], lhsT=PT_[0:rows, sub * 128:(sub + 1) * 128], rhs=Ra[0:rows, g, cch, :],
                                                          start=(ci_ == 0 and sub % 2 == 0), stop=(ci_ == nch - 1), skip_group_check=True), r=[PT_, Ra], w=[pOc])

                    last = (ci_ == len(chunks) - 1)
                    push_tile(score, expf, pv, after=(lambda g=g, hh=hh: post_head(g, hh)) if last else None)

    def B3(qg):
        par = qg % 2
        tg0 = qg * 512
        QA_, oacc_, gat_ = QA[par], oacc[par], gat[par]

        def post_branch(h, bi, pO_):
            rz_, gz_, ot_ = rz[bi % 2], gz[bi % 2], otmp[bi % 2]
            c.op("dve", lambda e: e.reciprocal(out=rz_[:], in_=pO_[:, :, 64]), r=[pO_], w=[rz_])
            c.op("dve", lambda e: e.tensor_tensor(out=gz_[:], in0=rz_[:], in1=gat_[:, :, 3 * h + bi], op=ALU.mult), r=[rz_, gat_], w=[gz_])
            c.op("dve", lambda e: e.tensor_tensor(out=ot_[:], in0=pO_[:, :, 0:64], in1=gz_[:].unsqueeze(2).to_broadcast([128, 4, 64]), op=ALU.mult), r=[pO_, gz_], w=[ot_])
            c.op("pool", lambda e: e.tensor_tensor(out=oacc_[:, :, h, :], in0=oacc_[:, :, h, :], in1=ot_[:], op=ALU.add), r=[("oacc", par, h), ot_], w=[("oacc", par, h)])
            if c.debug:
                dn_ = "s_dsel" if bi == 1 else "s_dwin"
                c.dma(D[dn_][tg0:tg0 + 512, h * 64:(h + 1) * 64].rearrange("(s p) d -> p s d", p=128), ot_[:], r=[ot_], w=[(dn_, qg, h)])
            if bi == 2 and h == 7:
                finish()

        def finish():
            ok = [("oacc", par, h) for h in range(8)]
            c.op("pool", lambda e: e.tensor_tensor(out=osq[:], in0=oacc_[:], in1=oacc_[:], op=ALU.mult), r=ok, w=[osq])
            c.op("dve", lambda e: e.tensor_reduce(out=ossq[:], in_=osq[:].rearrange("p s h d -> p (s h) d"), axis=AX.X, op=ALU.add), r=[osq], w=[ossq])
            rsqrt_ops(c, ossq[:], ossq[:], 1.0 / 64, [ossq], [ossq])
            c.op("dve", lambda e: e.tensor_tensor(out=osq[:].rearrange("p s h d -> p (s h) d"), in0=oacc_[:].rearrange("p s h d -> p (s h) d"),
                                                  in1=ossq[:].unsqueeze(2).to_broadcast([128, 32, 64]), op=ALU.mult), r=ok + [ossq], w=[osq])
            ya_ = yab[par]
            c.op("pool", lambda e: e.tensor_tensor(out=ya_[:], in0=osq[:].rearrange("p s h d -> p s (h d)"), in1=gout[:].unsqueeze(1).to_broadcast([128, 4, 512]), op=ALU.mult),
                 r=[osq, gout], w=[ya_])
            c.dma(D["s_ya"][tg0:tg0 + 512, :].rearrange("(s p) c -> p s c", p=128), ya_[:], q="sp", w=[("s_ya", qg * 4 + i) for i in range(4)])

        for h in range(8):
            g = h // 4
            qkeys = [("QAq", par), ("QAb", par, h)]
            nk = 4 * qg + 4
            for kt in range(nk):
                j = kt - 4 * qg
                c0 = 0 if j < 0 else 128 * j

                def score(pS_, g=g, h=h, kt=kt, j=j, c0=c0):
                    kcols = slice(kt * 128, (kt + 1) * 128)
                    if j < 0:
                        c.op("pe", lambda e: e.matmul(pS_[:, :], lhsT=KA[g][:, kcols], rhs=QA_[:, h, :], start=True, stop=True), r=[KA[g]] + qkeys, w=[pS_])
                    else:
                        c.op("pe", lambda e: e.matmul(pS_[:, c0:c0 + 128], lhsT=identb[:], rhs=trib[:], start=True, stop=False), r=[identb, trib], w=[pS_])
                        c.op("pe", lambda e: e.matmul(pS_[:, c0:c0 + 128], lhsT=KA[g][:, kcols], rhs=QA_[:, h, c0:c0 + 128], start=False, stop=True), r=[KA[g]] + qkeys, w=[pS_])
                        if c0 + 128 < 512:
                            c.op("pe", lambda e: e.matmul(pS_[:, c0 + 128:512], lhsT=KA[g][:, kcols], rhs=QA_[:, h, c0 + 128:512], start=True, stop=True), r=[KA[g]] + qkeys, w=[pS_])

                def expf(pS_, PT_, c0=c0):
                    c.op("act", lambda e: e.activation(out=PT_[:, c0:512], in_=pS_[:, c0:512], func=AF.Exp), r=[pS_], w=[PT_])

                def pv(PT_, g=g, kt=kt, c0=c0):
                    for sub in range(c0 // 128, 4):
                        c.op("pe", lambda e: e.matmul(pOs[:, sub, :], lhsT=PT_[:, sub * 128:(sub + 1) * 128], rhs=VSa[:, g, kt, :], start=(kt == 0 and sub == 0), stop=(kt == 4 * qg + sub), skip_group_check=True),
                             r=[PT_, VSa], w=[pOs])

                push_tile(score, expf, pv, after=(lambda h=h: post_branch(h, 1, pOs)) if kt == nk - 1 else None)
            tiles = [("lo", j) for j in range(4) if qg > 0] + [("up", j) for j in range(4)]
            for idx_, (kind, j) in enumerate(tiles):
                kt = 4 * qg - 4 + j if kind == "lo" else 4 * qg + j
                if kind == "lo":
                    c0, c1 = 0, 128 * (j + 1)
                    subs = list(range(0, j + 1))
                else:
                    c0, c1 = 128 * j, 512
                    subs = list(range(j, 4))

                def score(pS_, g=g, h=h, kt=kt, j=j, kind=kind, c0=c0):
                    kcols = slice(kt * 128, (kt + 1) * 128)
                    if kind == "lo":
                        m0_ = 128 * j
                        if j > 0:
                            c.op("pe", lambda e: e.matmul(pS_[:, 0:m0_], lhsT=KW[:, g, kcols], rhs=QA_[0:64, h, 0:m0_], start=True, stop=True), r=[KW, ("QAq", par)], w=[pS_])
                        c.op("pe", lambda e: e.matmul(pS_[:, m0_:m0_ + 128], lhsT=identb[:], rhs=lowb[:], start=True, stop=False), r=[identb, lowb], w=[pS_])
                        c.op("pe", lambda e: e.matmul(pS_[:, m0_:m0_ + 128], lhsT=KW[:, g, kcols], rhs=QA_[0:64, h, m0_:m0_ + 128], start=False, stop=True), r=[KW, ("QAq", par)], w=[pS_])
                    else:
                        c.op("pe", lambda e: e.matmul(pS_[:, c0:c0 + 128], lhsT=identb[:], rhs=trib[:], start=True, stop=False), r=[identb, trib], w=[pS_])
                        c.op("pe", lambda e: e.matmul(pS_[:, c0:c0 + 128], lhsT=KW[:, g, kcols], rhs=QA_[0:64, h, c0:c0 + 128], start=False, stop=True), r=[KW, ("QAq", par)], w=[pS_])
                        if c0 + 128 < 512:
                            c.op("pe", lambda e: e.matmul(pS_[:, c0 + 128:512], lhsT=KW[:, g, kcols], rhs=QA_[0:64, h, c0 + 128:512], start=True, stop=True), r=[KW, ("QAq", par)], w=[pS_])

                def expf(pS_, PT_, c0=c0, c1=c1):
                    c.op("act", lambda e: e.activation(out=PT_[:, c0:c1], in_=pS_[:, c0:c1], func=AF.Exp), r=[pS_], w=[PT_])

                def pv(PT_, g=g, kt=kt, kind=kind, j=j, subs=subs, first_tile=(idx_ == 0)):
                    for sub in subs:
                        first = first_tile and sub == subs[0]
                        last = (kind == "up" and j == sub)
                        c.op("pe", lambda e: e.matmul(pOw[:, sub, :], lhsT=PT_[:, sub * 128:(sub + 1) * 128], rhs=VWa[:, g, kt, :], start=first, stop=last, skip_group_check=True),
                             r=[PT_, VWa], w=[pOw])

                push_tile(score, expf, pv, after=(lambda h=h: post_branch(h, 2, pOw)) if idx_ == len(tiles) - 1 else None)

    for step in range(NG + 1):
        if step < NG:
            B2(step)
        if step >= 1:
            B3(step - 1)
            while stq:
                pop_tile()
    P.close()


def phase_C(c, L, D):
    K = c.K
    identf, identb = K["identf"], K["identb"]
    P = Pool_(c, "C%d" % L)
    GT = P.sb("GT", [128, 32, 3, 4], F32)
    decb = P.sb("decb", [128, 256], F32)
    P0 = Pool_(c, "C0_%d" % L)
    ifT = P0.sb("ifT", [128, 32, 8], F32)
    c.dma(ifT[:], D["s_if"].rearrange("(i p) c -> p i c", p=128), r=[("s_if", i) for i in range(32)])
    fb = P0.sb("fb", [4, 1], F32)
    c.dma(fb[:], D["m_fgate_b"][L].rearrange("(p o) -> p o", o=1))
    c.op("dve", lambda e: e.tensor_scalar(out=fb[:], in0=fb[:], scalar1=-1.0, scalar2=None, op0=ALU.mult), r=[fb], w=[fb])
    G = {n: P0.sb("G" + n, [4, T], F32) for n in ("I", "F", "X1", "X2", "KM", "RM", "CM", "NM")}
    pG = [P0.ps("pG%d" % i, [4, 4, 128], F32) for i in range(2)]
    n = 0
    for col, dst in ((0, G["I"]), (4, G["F"])):
        for i4 in range(8):
            pg = pG[n % 2]
            n += 1
            for j in range(4):
                it = i4 * 4 + j
                c.op("pe", lambda e: e.transpose(out=pg[:, j, :], in_=ifT[:, it, col:col + 4], identity=identf[:]), r=[ifT, identf], w=[pg])
            c.op("act", lambda e: e.copy(out=dst[:, i4 * 512:(i4 + 1) * 512], in_=pg[:].rearrange("p a b -> p (a b)")), r=[pg], w=[dst])
    c.op("pool", lambda e: e.memset(G["KM"][:], 1.0), w=[G["KM"]])
    c.op("pool", lambda e: e.memset(G["KM"][:, 0:T:64], 0.0), r=[G["KM"]], w=[G["KM"]])
    c.op("pool", lambda e: e.memset(G["RM"][:], 0.0), w=[G["RM"]])
    c.op("pool", lambda e: e.memset(G["RM"][:, 0:T:64], -1e30), r=[G["RM"]], w=[G["RM"]])
    c.op("act", lambda e: e.activation(out=G["X1"][:], in_=G["F"][:], func=AF.Exp, scale=-1.0, bias=fb[:, 0:1]), r=[G["F"], fb], w=[G["X1"]])
    c.op("act", lambda e: e.activation(out=G["X1"][:], in_=G["X1"][:], func=AF.Ln, bias=c.one_t[0:4, :]), r=[G["X1"], c.one_t], w=[G["X1"]])
    c.op("dve", lambda e: e.tensor_tensor_scan(out=G["X2"][:], data0=G["KM"][:], data1=G["X1"][:], initial=0.0, op0=ALU.mult, op1=ALU.add),
         r=[G["KM"], G["X1"]], w=[G["X2"]])
    c.op("dve", lambda e: e.tensor_tensor(out=G["I"][:], in0=G["I"][:], in1=G["X2"][:], op=ALU.add), r=[G["I"], G["X2"]], w=[G["I"]])
    c.op("dve", lambda e: e.tensor_tensor_scan(out=G["CM"][:], data0=G["RM"][:], data1=G["I"][:], initial=-1e30, op0=ALU.add, op1=ALU.max),
         r=[G["RM"], G["I"]], w=[G["CM"]])
    sm = {n: P0.sb("sm" + n, [4, 64], F32) for n in ("U", "B", "mn", "m", "d")}
    c.op("dve", lambda e: e.tensor_copy(out=sm["U"][:], in_=G["CM"][:, 63:T:64]), r=[G["CM"]], w=[sm["U"]])
    c.op("dve", lambda e: e.tensor_scalar(out=sm["B"][:], in0=G["X2"][:, 63:T:64], scalar1=-1.0, scalar2=None, op0=ALU.mult), r=[G["X2"]], w=[sm["B"]])
    c.op("dve", lambda e: e.tensor_tensor_scan(out=sm["mn"][:], data0=sm["U"][:], data1=sm["B"][:], initial=0.0, op0=ALU.max, op1=ALU.add),
         r=[sm["U"], sm["B"]], w=[sm["mn"]])
    c.op("dve", lambda e: e.memset(sm["m"][:, 0:1], 0.0), w=[sm["m"]])
    c.op("dve", lambda e: e.tensor_copy(out=sm["m"][:, 1:64], in_=sm["mn"][:, 0:63]), r=[sm["mn"], sm["m"]], w=[sm["m"]])
    v3 = lambda t_: t_[:].rearrange("p (c t) -> p c t", t=64)
    mb = sm["m"][:].unsqueeze(2).to_broadcast([4, 64, 64])
    c.op("dve", lambda e: e.tensor_tensor(out=v3(G["CM"]), in0=v3(G["CM"]), in1=mb, op=ALU.max), r=[G["CM"], sm["m"]], w=[G["CM"]])
    c.op("dve", lambda e: e.tensor_scalar(out=G["NM"][:], in0=G["CM"][:], scalar1=-1.0, scalar2=None, op0=ALU.mult), r=[G["CM"]], w=[G["NM"]])
    c.dma(D["s_mg"][0], G["NM"][:], w=[("s_mg", 0)])
    c.op("dve", lambda e: e.tensor_tensor(out=v3(G["X1"]), in0=v3(G["NM"]), in1=mb, op=ALU.add), r=[G["NM"], sm["m"], G["X1"]], w=[G["X1"]])
    c.op("act", lambda e: e.activation(out=G["F"][:], in_=G["X1"][:], func=AF.Exp), r=[G["X1"], G["F"]], w=[G["F"]])
    c.dma(D["s_mg"][1], G["F"][:], w=[("s_mg", 1)])
    c.op("dve", lambda e: e.tensor_copy(out=sm["d"][:], in_=G["F"][:, 63:T:64]), r=[G["F"]], w=[sm["d"]])
    c.dma(D["s_dec"], sm["d"][:], w=[("s_dec",)])
    c.dma(decb[:], D["s_dec"].rearrange("h c -> (h c)").partition_broadcast(128), r=[("s_dec",)], w=[decb])
    c.op("dve", lambda e: e.tensor_tensor(out=G["X1"][:], in0=G["X2"][:], in1=G["NM"][:], op=ALU.add), r=[G["X2"], G["NM"], G["X1"]], w=[G["X1"]])
    c.op("act", lambda e: e.activation(out=G["KM"][:], in_=G["X1"][:], func=AF.Exp), r=[G["X1"], G["KM"]], w=[G["KM"]])
    nm63 = G["NM"][:, 63:T:64].unsqueeze(2).to_broadcast([4, 64, 64])
    c.op("dve", lambda e: e.tensor_tensor(out=v3(G["X1"]), in0=v3(G["I"]), in1=nm63, op=ALU.add), r=[G["I"], G["NM"], G["X1"]], w=[G["X1"]])
    c.op("act", lambda e: e.activation(out=G["RM"][:], in_=G["X1"][:], func=AF.Exp), r=[G["X1"], G["RM"]], w=[G["RM"]])
    c.op("dve", lambda e: e.tensor_scalar(out=G["RM"][:], in0=G["RM"][:], scalar1=128.0 ** -0.5, scalar2=None, op0=ALU.mult), r=[G["RM"]], w=[G["RM"]])
    PK = P0.sb("PK", [128, T], F32)
    c.op("pool", lambda e: e.memset(PK[:], 0.0), w=[PK])
    for q, src in enumerate((G["I"], G["KM"], G["RM"])):
        c.dma(PK[32 * q:32 * q + 4, :], src[:], r=[src, PK], w=[PK])
    pP = [P0.ps("pP%d" % i, [128, 4, 128], F32) for i in range(2)]
    for i4 in range(8):
        pp = pP[i4 % 2]
        for j in range(4):
            it = i4 * 4 + j
            c.op("pe", lambda e: e.transpose(out=pp[:, j, :], in_=PK[:, it * 128:(it + 1) * 128], identity=identf[:]), r=[PK, identf], w=[pp])
        c.op("act", lambda e: e.copy(out=GT[:, i4 * 4:(i4 + 1) * 4, :, :], in_=pp[:, :, 0:96].rearrange("p j (q x) -> p j q x", x=32)[:, :, :, 0:4]), r=[pp, GT], w=[GT])
    P0.close()

    mBD = P.sb("mBD", [128, 128], F32)
    c.op("pool", lambda e: e.memset(mBD[:], -1e30), w=[mBD])
    c.dma(mBD[0:64, 0:64], D["c_mcaus"], r=[mBD], w=[mBD])
    c.dma(mBD[64:128, 64:128], D["c_mcaus"], r=[mBD], w=[mBD])
    gm = P.sb("gm", [128, 512], F32)
    c.dma(gm[:], D["m_out_norm_g"][L].partition_broadcast(128))
    nmb = [P.sb("nmb%d" % i, [128, 4, 512], F32) for i in range(2)]
    wib = [P.sb("wib%d" % i, [128, 4, 512], F32) for i in range(2)]
    vA = [P.sb("vA%d" % i, [128, 4, 4, 129], BF16) for i in range(2)]
    obt = [P.sb("obt%d" % i, [128, 4, 512], F32) for i in range(2)]
    ybt = [P.sb("ybt%d" % i, [128, 4, 512], BF16) for i in range(2)]
    qT = [[P.sb("qT%d_%d" % (i, h), [128, 512], BF16) for h in range(4)] for i in range(2)]
    kT = [[P.sb("kT%d_%d" % (i, h), [128, 512], BF16) for h in range(4)] for i in range(2)]
    q2 = [[P.sb("q2_%d_%d" % (i, h), [128, 2, 512], BF16) for h in range(4)] for i in range(2)]
    ksc = [[P.sb("ksc%d_%d" % (i, h), [128, 4, 128], BF16) for h in range(4)] for i in range(2)]
    wi = [[P.sb("wi%d_%d" % (i, h), [128, 4, 128], BF16) for h in range(4)] for i in range(2)]
    wt = [P.sb("wt%d" % i, [128, 4, 128], F32) for i in range(2)]
    Cf = [P.sb("Cf%d" % h, [128, 129], F32) for h in range(4)]
    Cb = [[P.sb("Cb%d_%d" % (h, i), [128, 129], BF16) for i in range(3)] for h in range(4)]
    ncb = [0, 0, 0, 0]
    nums = [P.sb("nums%d" % i, [128, 4, 4, 129], F32) for i in range(2)]
    psq = P.sb("psq", [128, 16, 128], F32)
    pd16 = [P.sb("pd16_%d" % i, [128, 16], F32) for i in range(4)]
    for i in range(2):
        for h in range(4):
            c.op("pool", lambda e: e.memset(q2[i][h][:], 0.0), w=[q2[i][h]])
        c.op("pool", lambda e: e.memset(vA[i][:, :, :, 128:129], 1.0), w=[vA[i]])
    for h in range(4):
        c.op("pool", lambda e: e.memset(Cf[h][:], 0.0), w=[Cf[h]])
        c.op("pool", lambda e: e.memset(Cb[h][0][:], 0.0), w=[Cb[h][0]])
    pSm = [P.ps("pSm%d" % i, [128, 4, 128], F32) for i in range(2)]
    pKt = [P.ps("pKt%d" % i, [128, 4, 128], BF16) for i in range(1)]
    pNm = [P.ps("pNm%d" % i, [128, 2, 129], F32) for i in range(2)]
    pDl = [P.ps("pDl%d" % i, [128, 2, 129], F32) for i in range(2)]
    cnt = {"sm": 0}

    def loads(cg):
        tg0 = cg * 512
        par = cg % 2
        nmb_, wib_, vA_, obt_ = nmb[par], wib[par], vA[par], obt[par]
        c.dma(nmb_[:], D["s_mg"][0, :, tg0:tg0 + 512].partition_broadcast(128), r=[("s_mg", 0)], w=[nmb_])
        c.dma(wib_[:], D["s_mg"][1, :, tg0:tg0 + 512].partition_broadcast(128), r=[("s_mg", 1)], w=[wib_])
        for ti in range(4):
            it = cg * 4 + ti
            c.dma(vA_[:, ti, :, 0:128], D["s_vb"][it * 128:(it + 1) * 128, :].rearrange("p (h e) -> p h e", e=128), r=[("s_vb", it)], w=[vA_])
        c.dma(obt_[:], D["s_ob"][tg0:tg0 + 512, :].rearrange("(s p) c -> p s c", p=128), r=[("s_ob", cg * 4 + i) for i in range(4)], w=[obt_])
        c.op("pool", lambda e: e.tensor_tensor(out=obt_[:], in0=obt_[:], in1=gm[:].unsqueeze(1).to_broadcast([128, 4, 512]), op=ALU.mult), r=[obt_, gm], w=[obt_])
        for h in range(4):
            c.dma(qT[par][h][:], D["s_qb"][h, :, tg0:tg0 + 512], r=[("s_qb", h, cg)])
            c.dma(kT[par][h][:], D["s_kb"][h, :, tg0:tg0 + 512], r=[("s_kb", h, cg)])

    def prep(cg, h):
        par = cg % 2
        nmb_, wib_ = nmb[par], wib[par]
        qT_, kT_, q2_, ksc_, wi_ = qT[par][h], kT[par][h], q2[par][h], ksc[par][h], wi[par][h]
        cnt["sm"] += 1
        pSm_, wt_, pKt_ = pSm[cnt["sm"] % 2], wt[cnt["sm"] % 2], pKt[0]
        for eo in range(2):
            vw = lambda a: a.rearrange("p (c two t) -> p c two t", two=2, t=64)[:, :, eo, :]
            c.op("dve", lambda e: e.tensor_tensor(out=vw(q2_[:, eo, :]), in0=vw(qT_[:]), in1=vw(wib_[:, h, :]), op=ALU.mult), r=[qT_, wib_, q2_], w=[q2_])
        for ti in range(4):
            cs = slice(ti * 128, (ti + 1) * 128)
            c.op("pe", lambda e: e.matmul(pSm_[:, ti, :], lhsT=kT_[:, cs], rhs=qT_[:, cs], start=True, stop=True), r=[kT_, qT_], w=[pSm_])
        for ti in range(4):
            cs = slice(ti * 128, (ti + 1) * 128)
            c.op("pe", lambda e: e.transpose(out=pKt_[:, ti, :], in_=kT_[:, cs], identity=identb[:]), r=[kT_, identb], w=[pKt_])
        for ti in range(4):
            it = cg * 4 + ti
            c.op("dve", lambda e: e.tensor_scalar(out=ksc_[:, ti, :], in0=pKt_[:, ti, :], scalar1=GT[:, it, 2, h:h + 1], scalar2=None, op0=ALU.mult), r=[pKt_, GT, ksc_], w=[ksc_])
        c.op("pool", lambda e: e.tensor_tensor(out=wt_[:], in0=nmb_[:, h, :].rearrange("p (j t) -> p j t", t=128), in1=mBD[:].unsqueeze(1).to_broadcast([128, 4, 128]), op=ALU.add),
             r=[nmb_, mBD], w=[wt_])
        for ti in range(4):
            it = cg * 4 + ti
            c.op("act", lambda e: e.activation(out=wt_[:, ti, :], in_=wt_[:, ti, :], func=AF.Exp, bias=GT[:, it, 0, h:h + 1]), r=[wt_, GT], w=[wt_])
        c.op("dve", lambda e: e.scalar_tensor_tensor(out=wi_[:].rearrange("p a b -> p (a b)"), in0=pSm_[:].rearrange("p a b -> p (a b)"), scalar=128.0 ** -0.5,
                                                     in1=wt_[:].rearrange("p a b -> p (a b)"), op0=ALU.mult, op1=ALU.mult), r=[wt_, pSm_], w=[wi_])

    def recur_tile(cg, ti):
        par = cg % 2
        vA_, nums_ = vA[par], nums[par]
        it = cg * 4 + ti
        cs = slice(ti * 128, (ti + 1) * 128)
        cbs = []
        for h in range(4):
            cbs.append((Cb[h][ncb[h] % 3], Cb[h][(ncb[h] + 1) % 3], Cb[h][(ncb[h] + 2) % 3]))
            ncb[h] += 2
        for half, (p0, p1) in enumerate(((0, 64), (64, 128))):
            ce = 2 * it + half
            for h in range(4):
                pd = pDl[h // 2]
                c.op("pe", lambda e: e.matmul(pd[:, h % 2, :], lhsT=ksc[par][h][p0:p1, ti, :], rhs=vA_[p0:p1, ti, h, :], start=True, stop=True),
                     r=[ksc[par][h], vA_], w=[(pd.name, h % 2)])
            for h in range(4):
                pd = pDl[h // 2]
                c.op("dve", lambda e: e.scalar_tensor_tensor(out=Cf[h][:], in0=Cf[h][:], scalar=decb[:, h * 64 + ce:h * 64 + ce + 1], in1=pd[:, h % 2, :], op0=ALU.mult, op1=ALU.add),
                     r=[Cf[h], decb, (pd.name, h % 2)], w=[Cf[h]])
                tgt = cbs[h][1] if half == 0 else cbs[h][2]
                c.op("act", lambda e: e.copy(out=tgt[:], in_=Cf[h][:]), r=[Cf[h]], w=[tgt])
            if half == 0:
                for h in range(4):
                    pn = pNm[h // 2]
                    q2_, wi_ = q2[par][h], wi[par][h]
                    cb_e, cb_o, _ = cbs[h]
                    c.op("pe", lambda e: e.matmul(pn[:, h % 2, :], lhsT=q2_[:, 0, cs], rhs=cb_e[:], start=True, stop=False), r=[q2_, cb_e], w=[(pn.name, h % 2)])
                    c.op("pe", lambda e: e.matmul(pn[:, h % 2, :], lhsT=q2_[:, 1, cs], rhs=cb_o[:], start=False, stop=False), r=[q2_, cb_o], w=[(pn.name, h % 2)])
                    c.op("pe", lambda e: e.matmul(pn[:, h % 2, :], lhsT=wi_[:, ti, :], rhs=vA_[:, ti, h, :], start=False, stop=True), r=[wi_, vA_], w=[(pn.name, h % 2)])
        for hp in range(2):
            pn = pNm[hp]
            c.op("act", lambda e: e.copy(out=nums_[:, ti, 2 * hp:2 * hp + 2, :], in_=pn[:]), r=[(pn.name, 0), (pn.name, 1)], w=[("nums", par, ti, hp)])

    def post(cg):
        par = cg % 2
        tg0 = cg * 512
        nums_, obt_, ybt_ = nums[par], obt[par], ybt[par]
        nk = [("nums", par, ti, hp) for ti in range(4) for hp in range(2)]
        v16 = lambda t_: t_[:].rearrange("p (a b) -> p a b", b=4)
        d0, d1, d2, d3 = pd16
        c.op("act", lambda e: e.activation(out=v16(d0), in_=nums_[:, :, :, 128], func=AF.Abs), r=nk, w=[d0])
        c.op("dve", lambda e: e.tensor_tensor(out=v16(d0), in0=v16(d0), in1=GT[:, cg * 4:(cg + 1) * 4, 1, :], op=ALU.max), r=[d0, GT], w=[d0])
        c.op("dve", lambda e: e.reciprocal(out=d0[:], in_=d0[:]), r=[d0], w=[d0])
        nv = nums_[:].rearrange("p a b e -> p (a b) e")[:, :, 0:128]
        c.op("pool", lambda e: e.tensor_tensor(out=psq[:], in0=nv, in1=nv, op=ALU.mult), r=nk, w=[psq])
        c.op("dve", lambda e: e.tensor_reduce(out=d1[:], in_=psq[:], axis=AX.X, op=ALU.add), r=[psq], w=[d1])
        c.op("dve", lambda e: e.tensor_tensor(out=d1[:], in0=d1[:], in1=d0[:], op=ALU.mult), r=[d1, d0], w=[d1])
        c.op("dve", lambda e: e.tensor_tensor(out=d1[:], in0=d1[:], in1=d0[:], op=ALU.mult), r=[d1, d0], w=[d1])
        rsqrt_ops(c, d2[:], d1[:], 1.0 / 128, [d1], [d2])
        c.op("dve", lambda e: e.tensor_tensor(out=d3[:], in0=d0[:], in1=d2[:], op=ALU.mult), r=[d0, d2], w=[d3])
        c.op("dve", lambda e: e.tensor_tensor(out=psq[:], in0=nv, in1=d3[:].unsqueeze(2).to_broadcast([128, 16, 128]), op=ALU.mult), r=nk + [d3, psq], w=[psq])
        c.op("pool", lambda e: e.tensor_tensor(out=ybt_[:], in0=psq[:].rearrange("p (a b) e -> p a (b e)", b=4), in1=obt_[:], op=ALU.mult), r=[psq, obt_], w=[ybt_])
        c.dma(D["s_yb"][tg0:tg0 + 512, :].rearrange("(s p) c -> p s c", p=128), ybt_[:], q="sp", w=[("s_yb", cg * 4 + i) for i in range(4)])

    loads(0)
    for h in range(4):
        prep(0, h)
    for cg in range(NG):
        if cg + 1 < NG:
            loads(cg + 1)
        for ti in range(4):
            recur_tile(cg, ti)
            if cg + 1 < NG:
                prep(cg + 1, ti)
        post(cg)
    P.close()


_CONSTS = None


def kernel(**inputs):
    global _CONSTS
    if _CONSTS is None:
        _CONSTS = host_consts()
    nc = build_program()
    x = np.ascontiguousarray(inputs["x"], dtype=np.float32)
    base = {k: np.ascontiguousarray(inputs[k], dtype=np.float32) for k in WEIGHT_SHAPES}
    base.update(_CONSTS)
    in_maps = []
    for b in range(8):
        m = dict(base)
        m["x"] = x[b]
        in_maps.append(m)
    res = run_bass_kernel_spmd(nc, in_maps, core_ids=list(range(8)))
    return np.stack([res.results[b]["out"] for b in range(8)], axis=0).astype(np.float32)
```

```python
import numpy as np
import concourse.bass as bass
import concourse.mybir as mybir
from concourse.bass_utils import run_bass_kernel_spmd
from contextlib import ExitStack

F32 = mybir.dt.float32
BF16 = mybir.dt.bfloat16
ALU = mybir.AluOpType
AF = mybir.ActivationFunctionType
AX = mybir.AxisListType

T = 4096
DM = 1024
DEPTH = 2
DIN = 3360
FF = 2816
NG = 8
EPS = 1e-6
NEG = -30000.0
NDMA_SEMS = 24

O_QA, O_KC, O_VC, O_KS, O_VS, O_KW, O_VW, O_GA = 0, 512, 640, 768, 896, 1024, 1152, 1280
O_QB, O_KB, O_VB, O_IF, O_OB = 1304, 1816, 2328, 2840, 2848


class Ctx:
    def __init__(self, nc, es):
        self.nc = nc
        self.engs = {"pe": nc.tensor, "act": nc.scalar, "dve": nc.vector, "pool": nc.gpsimd, "sp": nc.sync}
        self.sem = {k: es.enter_context(nc.semaphore("s_" + k)) for k in self.engs}
        self.cnt = {k: 0 for k in self.engs}
        self.seen = {k: {} for k in self.engs}
        self.dsem = [es.enter_context(nc.semaphore("s_dma%d" % i)) for i in range(NDMA_SEMS)]
        self.dcnt = [0] * NDMA_SEMS
        self.dnext = 0
        self.res = {}
        self.n_wait = 0
        self.n_ins = 0
        self.uid = 0

    def _semh(self, key):
        return self.sem[key] if isinstance(key, str) else self.dsem[key[1]]

    def _wait(self, eng, tok):
        key, val = tok
        if self.seen[eng].get(key, 0) >= val:
            return
        if key == eng and eng == "pe":
            return
        self.engs[eng].wait_ge(self._semh(key), val)
        self.seen[eng][key] = val
        self.n_wait += 1

    @staticmethod
    def _key(a):
        if isinstance(a, (str, tuple)):
            return a
        t = getattr(a, "tensor", None)
        return t.name if t is not None else a.name

    def _deps(self, eng, r, w):
        toks = []
        for a in r:
            ent = self.res.get(self._key(a))
            if ent and ent[0] is not None:
                toks.append(ent[0])
        for a in w:
            ent = self.res.get(self._key(a))
            if ent:
                if ent[0] is not None:
                    toks.append(ent[0])
                toks.extend(ent[1])
        for t in toks:
            self._wait(eng, t)

    def _record(self, tok, r, w):
        for a in r:
            ent = self.res.setdefault(self._key(a), [None, []])
            ent[1] = [t for t in ent[1] if t[0] != tok[0]] + [tok]
        for a in w:
            self.res[self._key(a)] = [tok, []]

    def op(self, eng, fn, r=(), w=()):
        self._deps(eng, r, w)
        ins = fn(self.engs[eng])
        self.cnt[eng] += 1
        ins.then_inc(self.sem[eng], 1)
        self._record((eng, self.cnt[eng]), r, w)
        self.n_ins += 1
        return ins

    def dma(self, out, in_, q="sp", r=None, w=None, **kw):
        r = [in_] if r is None else r
        w = [out] if w is None else w
        self._deps(q, r, w)
        i = self.dnext
        self.dnext = (self.dnext + 1) % NDMA_SEMS
        if self.dcnt[i] > 0:
            self._wait(q, (("d", i), self.dcnt[i]))
        ins = self.engs[q].dma_start(out=out, in_=in_, **kw)
        self.dcnt[i] += 16
        ins.then_inc(self.dsem[i], 16)
        self._record((("d", i), self.dcnt[i]), r, w)
        self.n_ins += 1
        return ins

    def barrier(self, engs=None):
        for e in (engs or self.engs):
            for k in self.engs:
                if self.cnt[k] > 0:
                    self._wait(e, (k, self.cnt[k]))
            for i in range(NDMA_SEMS):
                if self.dcnt[i] > 0:
                    self._wait(e, (("d", i), self.dcnt[i]))
        self.res = {}


class Pool_:
    def __init__(self, c, tag):
        self.c = c
        self.tag = tag
        self.es = ExitStack()

    def sb(self, name, shape, dt):
        self.c.uid += 1
        return self.es.enter_context(self.c.nc.sbuf_tensor("%s_%s_%d" % (self.tag, name, self.c.uid), shape, dt))

    def ps(self, name, shape, dt):
        self.c.uid += 1
        return self.es.enter_context(self.c.nc.psum_tensor("%s_%s_%d" % (self.tag, name, self.c.uid), shape, dt))

    def close(self, keep=()):
        self.c.barrier()
        self.es.close()


def host_consts():
    p = np.arange(128)
    cst = {}
    cst["c_ident"] = np.eye(128, dtype=np.float32)
    bo = np.zeros((128, 128), np.float32)
    bo[:64, :64] = 1.0
    bo[64:, 64:] = 1.0
    cst["c_bones"] = bo
    cst["c_tri"] = np.where(p[:, None] > p[None, :], NEG, 0.0).astype(np.float32)
    cst["c_low"] = np.where(p[None, :] >= p[:, None], NEG, 0.0).astype(np.float32)
    s = np.arange(T)
    cst["c_E"] = (s[None, :] // 64 == np.arange(64)[:, None]).astype(np.float32)
    t = np.arange(2560)
    cst["c_m0"] = np.where(t[None, :] < 16 * p[:, None] + 31, NEG, 0.0).astype(np.float32)
    n = np.arange(256)
    blk = np.arange(64)
    cs = n * 16
    ce = cs + 31
    ov = ((cs[:, None] < (blk[None, :] + 1) * 64) & (ce[:, None] >= blk[None, :] * 64)).astype(np.float32)
    ov[255] = 0.0
    cst["c_ov"] = ov
    tt = np.arange(T)
    cur = tt // 64
    forced = (blk[None] == 0) | (blk[None] == cur[:, None]) | (blk[None] == cur[:, None] - 1)
    valid = blk[None] * 64 <= tt[:, None]
    cst["c_bonus"] = np.where(valid, 1e4 * forced.astype(np.float32), -1e30).astype(np.float32)
    q = np.arange(64)
    cst["c_mcaus"] = np.where(q[:, None] > q[None, :], -1e30, 0.0).astype(np.float32)
    return cst


CONST_SHAPES = {"c_ident": [128, 128], "c_bones": [128, 128], "c_tri": [128, 128], "c_low": [128, 128],
                "c_E": [64, T], "c_m0": [128, 2560], "c_ov": [256, 64], "c_bonus": [T, 64],
                "c_mcaus": [64, 64]}

WEIGHT_SHAPES = {
    "ln1_g": [2, 1024], "w_in": [2, 1024, 3360], "b_in": [2, 3360], "nsa_q_norm_g": [2, 64],
    "nsa_k_norm_g": [2, 3, 64], "cmp_pe": [2, 2, 32, 64], "cmp_w1": [2, 2, 2048, 128],
    "cmp_w2": [2, 2, 128, 64], "m_conv_w": [2, 4, 1024], "m_conv_b": [2, 1024], "m_fgate_b": [2, 4],
    "nsa_out_norm_g": [2, 512], "m_out_norm_g": [2, 512], "w_out": [2, 1024, 1024], "ln2_g": [2, 1024],
    "w_gate_up": [2, 1024, 5632], "w_down": [2, 2816, 1024],
}

SCRATCH = {
    "s_qa": ([8, 64, T], BF16), "s_ks": ([2, 64, T], BF16), "s_kw": ([2, 64, T], BF16),
    "s_kc": ([128, T + 16], BF16), "s_vc": ([128, T + 16], BF16),
    "s_vs": ([T, 128], BF16), "s_vw": ([T, 128], BF16), "s_ga": ([T, 24], F32),
    "s_qb": ([4, 128, T], BF16), "s_kb": ([4, 128, T], BF16), "s_vb": ([T, 512], BF16),
    "s_ob": ([T, 512], F32), "s_if": ([T, 8], F32),
    "s_ocmp": ([T, 512], F32), "s_biasT": ([2, 64, T], BF16),
    "s_ya": ([T, 512], BF16), "s_yb": ([T, 512], BF16),
    "s_x1": ([T, 1024], F32), "s_x2": ([T, 1024], F32),
    "s_g1": ([256, 64], F32), "s_g2": ([256, 64], F32), "s_g3": ([256, 64], F32), "s_g4": ([256, 4], F32),
    "s_g5": ([4, 64], F32),
    "s_dsel": ([T, 512], F32), "s_dwin": ([T, 512], F32),
    "s_mg": ([2, 4, T], F32), "s_dec": ([4, 64], F32),
}


def rsqrt_ops(c, out, in_, scale, r, w, tmp=None):
    c.op("act", lambda e: e.activation(out=out, in_=in_, func=AF.Sqrt, scale=scale, bias=c.eps_ap[0:out.shape[0], :]), r=r + [c.eps_t], w=w)
    c.op("dve", lambda e: e.reciprocal(out=out, in_=out), r=w, w=w)


def phase_A(c, L, D, xin):
    nc = c.nc
    P = Pool_(c, "A%d" % L)
    K = c.K
    win = P.sb("win", [128, 8, DIN], BF16)
    stg = [P.sb("stg%d" % i, [128, 8, 512], F32) for i in range(2)]
    g1 = P.sb("g1", [128, 8], F32)
    c.dma(g1[:], D["ln1_g"][L].rearrange("(kc p) -> p kc", p=128), allow_slow_non_contiguous=True)
    wsrc = D["w_in"][L].rearrange("(k p) n -> p k n", p=128)
    wblocks = []
    wstate = {"next": 0}

    def load_wblock():
        i = wstate["next"]
        if i >= len(wblocks):
            return
        wstate["next"] += 1
        col, n = wblocks[i]
        s_ = stg[i % 2]
        c.dma(s_[:, :, 0:n], wsrc[:, :, col:col + n], w=[s_])
        eng = "dve" if i % 2 == 0 else "pool"
        c.op(eng, lambda e: e.tensor_tensor(out=win[:, :, col:col + n], in0=s_[:, :, 0:n], in1=g1[:].unsqueeze(2).to_broadcast([128, 8, n]), op=ALU.mult),
             r=[s_, g1], w=[("win", col)])
    fm = [("qa", O_QA + 128 * i, i) for i in range(4)] + [("kc", O_KC, 0), ("vc", O_VC, 0), ("ks", O_KS, 0), ("kw", O_KW, 0)]
    fm += [("qb", O_QB + 128 * i, i) for i in range(4)] + [("kb", O_KB + 128 * i, i) for i in range(4)]
    nfm = len(fm)
    tmg = [(O_VB, 512), (O_OB, 512), (O_VS, 128), (O_VW, 152), (O_IF, 8)]
    wblocks.extend([(col, 128) for (_, col, _) in fm] + tmg)
    bfm = P.sb("bfm", [128, nfm], F32)
    for ci, (_, col, _) in enumerate(fm):
        c.dma(bfm[:, ci:ci + 1], D["b_in"][L, col:col + 128].rearrange("(p o) -> p o", o=1), w=[bfm])
    gn = P.sb("gn", [128, 3], F32)
    for half in range(2):
        ps_ = slice(half * 64, half * 64 + 64)
        c.dma(gn[ps_, 0:1], D["nsa_q_norm_g"][L].rearrange("(p o) -> p o", o=1), w=[gn])
        c.dma(gn[ps_, 1:2], D["nsa_k_norm_g"][L, 1].rearrange("(p o) -> p o", o=1), w=[gn])
        c.dma(gn[ps_, 2:3], D["nsa_k_norm_g"][L, 2].rearrange("(p o) -> p o", o=1), w=[gn])
    c.op("dve", lambda e: e.tensor_scalar(out=gn[:, 0:1], in0=gn[:, 0:1], scalar1=0.125, scalar2=None, op0=ALU.mult), r=[gn], w=[gn])
    cw = P.sb("cw", [128, 8, 4], F32)
    cb = P.sb("cb", [128, 8], F32)
    for j in range(4):
        c.dma(cw[:, :, j], D["m_conv_w"][L, j].rearrange("(c p) -> p c", p=128), w=[cw], allow_slow_non_contiguous=True)
    c.dma(cb[:], D["m_conv_b"][L].rearrange("(c p) -> p c", p=128), allow_slow_non_contiguous=True)
    dg = P.sb("dg", [128, 8, 4, 128], BF16)
    for cc in range(8):
        for j in range(4):
            c.op("dve" if (cc + j) % 2 == 0 else "pool", lambda e: e.tensor_scalar(out=dg[:, cc, j, :], in0=K["identf"][:], scalar1=cw[:, cc, j:j + 1], scalar2=None, op0=ALU.mult),
                 r=[K["identf"], cw], w=[("dg", cc, j)])
    btm = P.sb("btm", [128, DIN], F32)
    c.dma(btm[:], D["b_in"][L].partition_broadcast(128))
    pre = P.sb("pre", [128, 8, 515], BF16)
    c.op("pool", lambda e: e.memset(pre[:], 0.0), w=[("pre", i) for i in range(8)])

    xt = [P.sb("xt%d" % i, [128, DM], F32) for i in range(2)]
    junk = P.sb("junk", [128, DM], BF16)
    ss = [P.sb("ss%d" % i, [128, 1], F32) for i in range(2)]
    hb = [P.sb("hb%d" % i, [128, DM], BF16) for i in range(2)]
    hT = [P.sb("hT%d" % i, [128, 8, 512], BF16) for i in range(2)]
    yf = [P.sb("yf%d" % i, [128, 512], F32) for i in range(2)]
    sq = [P.sb("sq%d" % i, [128, 512], BF16) for i in range(2)]
    rs = [P.sb("rs%d" % i, [128, 512], F32) for i in range(2)]
    ob16 = [P.sb("ob16_%d" % i, [128, 512], BF16) for i in range(4)]
    tv = [P.sb("tv%d" % i, [128, 512], BF16) for i in range(2)]
    to = [P.sb("to%d" % i, [128, 512], F32) for i in range(2)]
    tm = [P.sb("tm%d" % i, [128, 32], F32) for i in range(2)]
    tmb = [P.sb("tmb%d" % i, [128, 256], BF16) for i in range(2)]
    pT = [P.ps("pT%d" % i, [128, 8, 128], BF16) for i in range(2)]
    pF = [P.ps("pF%d" % i, [128, 512], F32) for i in range(2)]
    pN = P.ps("pN", [128, 512], F32)
    pV = P.ps("pV", [128, 512], F32)
    pX = [P.ps("pX%d" % i, [128, 512], F32) for i in range(2)]

    n_ob = [0]
    load_wblock()
    load_wblock()

    def stage2(g, ci, kind, idx, pF_):
        tg0 = g * 512
        o16 = ob16[n_ob[0] % 4]
        n_ob[0] += 1
        if kind in ("qa", "ks", "kw"):
            yf_, sq_, rs_ = yf[ci % 2], sq[ci % 2], rs[ci % 2]
            c.op("pe", lambda e: e.matmul(pN[:], lhsT=K["bonesb"][:], rhs=sq_[:], start=True, stop=True), r=[sq_, K["bonesb"]], w=[pN])
            rsqrt_ops(c, rs_[:], pN[:], 1.0 / 64, [pN], [rs_])
            gcol = {"qa": 0, "ks": 1, "kw": 2}[kind]
            c.op("dve", lambda e: e.scalar_tensor_tensor(out=o16[:], in0=yf_[:], scalar=gn[:, gcol:gcol + 1], in1=rs_[:], op0=ALU.mult, op1=ALU.mult),
                 r=[yf_, gn, rs_], w=[o16])
            if kind == "qa":
                dst = D["s_qa"][2 * idx:2 * idx + 2, :, tg0:tg0 + 512].rearrange("h p t -> (h p) t")
                wk = [("s_qa", 2 * idx, g), ("s_qa", 2 * idx + 1, g)]
            else:
                nm = "s_" + kind
                dst = D[nm][:, :, tg0:tg0 + 512].rearrange("h p t -> (h p) t")
                wk = [(nm, 0, g), (nm, 1, g)]
            c.dma(dst, o16[:], q="pool", w=wk)
        else:
            cc = idx + (0 if kind == "qb" else 4)
            pk = ("pre", cc)
            for j in range(4):
                c.op("pe", lambda e: e.matmul(pV[:], lhsT=dg[:, cc, j, :], rhs=pre[:, cc, j:j + 512], start=(j == 0), stop=(j == 3)), r=[pk, ("dg", cc, j)], w=[pV])
            c.op("pool", lambda e: e.tensor_copy(out=pre[:, cc, 0:3], in_=pre[:, cc, 512:515]), r=[pk], w=[pk])
            c.op("act", lambda e: e.activation(out=o16[:], in_=pV[:], func=AF.Silu, bias=cb[:, cc:cc + 1]), r=[pV, cb], w=[o16])
            nm = "s_" + kind
            c.dma(D[nm][idx, :, tg0:tg0 + 512], o16[:], q="pool", w=[(nm, idx, g)])

    for g in range(NG):
        hTg = hT[g % 2]
        for ti in range(4):
            it = g * 4 + ti
            x_ = xt[it % 2]
            ss_ = ss[it % 2]
            hb_ = hb[it % 2]
            pT_ = pT[it % 2]
            t0 = it * 128
            c.dma(x_[:], xin[t0:t0 + 128, :], r=[("x", it)])
            c.op("act", lambda e: e.activation(out=junk[:], in_=x_[:], func=AF.Square, accum_out=ss_[:]), r=[x_], w=[junk, ss_])
            rsqrt_ops(c, ss_[:], ss_[:], 1.0 / DM, [ss_], [ss_])
            c.op("dve", lambda e: e.tensor_scalar(out=hb_[:], in0=x_[:], scalar1=ss_[:, 0:1], scalar2=None, op0=ALU.mult), r=[x_, ss_], w=[hb_])
            for k in range(8):
                c.op("pe", lambda e: e.transpose(out=pT_[:, k, :], in_=hb_[:, k * 128:(k + 1) * 128], identity=K["identb"][:]), r=[hb_, K["identb"]], w=[pT_])
            c.op("act", lambda e: e.copy(out=hTg[:, :, ti * 128:(ti + 1) * 128], in_=pT_[:]), r=[pT_], w=[hTg])
        tg0 = g * 512
        pending = None
        for ci, (kind, col, idx) in enumerate(fm):
            pF_ = pF[ci % 2]
            if g == 0:
                load_wblock()
            for k in range(8):
                c.op("pe", lambda e: e.matmul(pF_[:], lhsT=win[:, k, col:col + 128], rhs=hTg[:, k, :], start=(k == 0), stop=(k == 7)),
                     r=[hTg, ("win", col)], w=[pF_])
            bias = bfm[:, ci:ci + 1]
            if kind in ("qa", "ks", "kw"):
                yf_, sq_ = yf[ci % 2], sq[ci % 2]
                c.op("act", lambda e: e.activation(out=yf_[:], in_=pF_[:], func=AF.Identity, bias=bias), r=[pF_, bfm], w=[yf_])
                c.op("act", lambda e: e.activation(out=sq_[:], in_=pF_[:], func=AF.Square, bias=bias), r=[pF_, bfm], w=[sq_])
            elif kind in ("kc", "vc"):
                o16 = ob16[n_ob[0] % 4]
                n_ob[0] += 1
                c.op("act", lambda e: e.activation(out=o16[:], in_=pF_[:], func=AF.Identity, bias=bias), r=[pF_, bfm], w=[o16])
                nm = "s_" + kind
                c.dma(D[nm][:, tg0:tg0 + 512], o16[:], q="pool", w=[(nm, g)])
            else:
                cc = idx + (0 if kind == "qb" else 4)
                c.op("act", lambda e: e.activation(out=pre[:, cc, 3:515], in_=pF_[:], func=AF.Identity, bias=bias), r=[pF_, bfm], w=[("pre", cc)])
            if pending is not None:
                stage2(*pending)
            pending = (g, ci, kind, idx, pF_) if kind not in ("kc", "vc") else None
        if g == 0:
            while wstate["next"] < len(wblocks):
                load_wblock()
        for ti in range(4):
            it = g * 4 + ti
            t0 = it * 128
            lt = lambda k: hTg[:, k, ti * 128:(ti + 1) * 128]

            def proj(pp, po, col, n):
                for k in range(8):
                    c.op("pe", lambda e: e.matmul(pp[:, po:po + n], lhsT=lt(k), rhs=win[:, k, col:col + n], start=(k == 0), stop=(k == 7)),
                         r=[hTg, ("win", col)], w=[(pp.name, po)])

            tv_, to_, tm_, tmb_ = tv[it % 2], to[it % 2], tm[it % 2], tmb[it % 2]
            proj(pX[0], 0, O_VB, 512)
            proj(pX[1], 0, O_OB, 512)
            if ti == 0 and pending is not None:
                stage2(*pending)
                pending = None
            c.op("dve", lambda e: e.tensor_tensor(out=tv_[:], in0=pX[0][:], in1=btm[:, O_VB:O_VB + 512], op=ALU.add), r=[(pX[0].name, 0), btm], w=[tv_])
            c.dma(D["s_vb"][t0:t0 + 128, :], tv_[:], q="pool", w=[("s_vb", it)])
            c.op("dve", lambda e: e.tensor_tensor(out=to_[:], in0=pX[1][:], in1=btm[:, O_OB:O_OB + 512], op=ALU.add), r=[(pX[1].name, 0), btm], w=[to_])
            c.op("act", lambda e: e.activation(out=to_[:], in_=to_[:], func=AF.Sigmoid), r=[to_], w=[to_])
            c.dma(D["s_ob"][t0:t0 + 128, :], to_[:], q="pool", w=[("s_ob", it)])
            pck = [(pX[0].name, 0), (pX[0].name, 128), (pX[0].name, 280)]
            for (po, col, n) in ((0, O_VS, 128), (128, O_VW, 152), (280, O_IF, 8)):
                for k in range(8):
                    c.op("pe", lambda e: e.matmul(pX[0][:, po:po + n], lhsT=lt(k), rhs=win[:, k, col:col + n], start=(k == 0), stop=(k == 7)),
                         r=[hTg, ("win", col)], w=pck)
            c.op("dve", lambda e: e.tensor_tensor(out=tmb_[:, 0:128], in0=pX[0][:, 0:128], in1=btm[:, O_VS:O_VS + 128], op=ALU.add), r=pck + [btm], w=[tmb_])
            c.op("dve", lambda e: e.tensor_tensor(out=tmb_[:, 128:256], in0=pX[0][:, 128:256], in1=btm[:, O_VW:O_VW + 128], op=ALU.add), r=pck + [btm, tmb_], w=[tmb_])
            c.op("dve", lambda e: e.tensor_tensor(out=tm_[:, 0:24], in0=pX[0][:, 256:280], in1=btm[:, O_GA:O_GA + 24], op=ALU.add), r=pck + [btm], w=[tm_])
            c.op("dve", lambda e: e.tensor_tensor(out=tm_[:, 24:32], in0=pX[0][:, 280:288], in1=btm[:, O_IF:O_IF + 8], op=ALU.add), r=pck + [btm, tm_], w=[tm_])
            c.op("act", lambda e: e.activation(out=tm_[:, 0:24], in_=tm_[:, 0:24], func=AF.Sigmoid), r=[tm_], w=[tm_])
            c.dma(D["s_vs"][t0:t0 + 128, :], tmb_[:, 0:128], q="pool", w=[("s_vs", it)])
            c.dma(D["s_vw"][t0:t0 + 128, :], tmb_[:, 128:256], q="pool", w=[("s_vw", it)])
            c.dma(D["s_ga"][t0:t0 + 128, :], tm_[:, 0:24], q="pool", w=[("s_ga", it)])
            c.dma(D["s_if"][t0:t0 + 128, :], tm_[:, 24:32], q="pool", w=[("s_if", it)])
    P.close()


def load_weight_bf16(c, P, dst, src_rows, nk, ncols, stg, scale=None, key="w"):
    for k in range(nk):
        s = stg[k % len(stg)]
        c.dma(s[:, 0:ncols], src_rows(k))
        eng = "dve" if k % 2 == 0 else "pool"
        if scale is None:
            c.op(eng, lambda e: e.tensor_copy(out=dst[:, k, :], in_=s[:, 0:ncols]), r=[s], w=[(key, k)])
        else:
            c.op(eng, lambda e: e.tensor_scalar(out=dst[:, k, :], in0=s[:, 0:ncols], scalar1=scale[:, k:k + 1], scalar2=None, op0=ALU.mult),
                 r=[s, scale], w=[(key, k)])


def phase_D(c, L, D, xin, EW):
    K = c.K
    P = Pool_(c, "D%d" % L)
    wo = P.sb("wo", [128, 8, DM], BF16)
    stg = [P.sb("stg%d" % i, [128, DM], F32) for i in range(2)]
    load_weight_bf16(c, P, wo, lambda k: D["w_out"][L, k * 128:(k + 1) * 128, :], 8, DM, stg, key="wo")
    xt = [P.sb("xt%d" % i, [128, DM], F32) for i in range(2)]
    yc = [P.sb("yc%d" % i, [128, DM], BF16) for i in range(2)]
    yT = [P.sb("yT%d" % i, [128, 8, 128], BF16) for i in range(2)]
    pT = [P.ps("pT%d" % i, [128, 8, 128], BF16) for i in range(2)]
    pO = [P.ps("pO%d" % i, [128, 2, 512], F32) for i in range(2)]
    for it in range(32):
        t0 = it * 128
        x_, yc_, yT_, pT_, pO_ = xt[it % 2], yc[it % 2], yT[it % 2], pT[it % 2], pO[it % 2]
        c.dma(x_[:], xin[t0:t0 + 128, :], r=[("x", it)])
        c.dma(yc_[:, 0:512], D["s_ya"][t0:t0 + 128, :], r=[("s_ya", it)], w=[yc_])
        c.dma(yc_[:, 512:1024], D["s_yb"][t0:t0 + 128, :], r=[("s_yb", it)], w=[yc_])
        for k in range(8):
            c.op("pe", lambda e: e.transpose(out=pT_[:, k, :], in_=yc_[:, k * 128:(k + 1) * 128], identity=K["identb"][:]), r=[yc_, K["identb"]], w=[pT_])
        c.op("act", lambda e: e.copy(out=yT_[:], in_=pT_[:]), r=[pT_], w=[yT_])
        for hf in range(2):
            for k in range(8):
                c.op("pe", lambda e: e.matmul(pO_[:, hf, :], lhsT=yT_[:, k, :], rhs=wo[:, k, hf * 512:(hf + 1) * 512], start=(k == 0), stop=(k == 7)),
                     r=[yT_, ("wo", k)], w=[pO_])
        c.op("dve", lambda e: e.tensor_tensor(out=x_[:], in0=x_[:], in1=pO_[:].rearrange("p a b -> p (a b)"), op=ALU.add), r=[x_, pO_], w=[x_])
        c.dma(D["s_x1"][t0:t0 + 128, :], x_[:], q="pool", w=[("x1", it)])
        if EW is not None:
            EW["load_gu"]()
            EW["load_wd"]()
    P.close(keep=[k for k in c.res if isinstance(k, tuple) and k[0] in ("wgu", "wd", "x1")] + ([EW["g2"].name] if EW else []))


def phase_E_weights(c, L, D):
    P = Pool_(c, "EW%d" % L)
    wgu = P.sb("wgu", [128, 22, 8, 256], BF16)
    wd = P.sb("wd", [128, 22, DM], BF16)
    stg = [P.sb("stg%d" % i, [128, 8, 256], F32) for i in range(2)]
    g2 = P.sb("g2", [128, 8], F32)
    c.dma(g2[:], D["ln2_g"][L].rearrange("(kc p) -> p kc", p=128), allow_slow_non_contiguous=True)
    gsrc = D["w_gate_up"][L].rearrange("(k p) n -> p k n", p=128)
    wst = {"n": 0, "gu": 0, "wd": 0}

    def load_gu():
        fc = wst["gu"]
        if fc >= 22:
            return
        wst["gu"] += 1
        s_ = stg[wst["n"] % 2]
        eng = "dve" if wst["n"] % 2 == 0 else "pool"
        wst["n"] += 1
        c.dma(s_[:, :, 0:128], gsrc[:, :, fc * 128:(fc + 1) * 128], w=[(s_.name, 0)])
        c.dma(s_[:, :, 128:256], gsrc[:, :, FF + fc * 128:FF + (fc + 1) * 128], w=[(s_.name, 1)])
        c.op(eng, lambda e: e.tensor_tensor(out=wgu[:, fc, :, :], in0=s_[:], in1=g2[:].unsqueeze(2).to_broadcast([128, 8, 256]), op=ALU.mult),
             r=[(s_.name, 0), (s_.name, 1), g2], w=[("wgu", fc)])

    def load_wd():
        k = wst["wd"]
        if k >= 22:
            return
        wst["wd"] += 1
        s_ = stg[wst["n"] % 2]
        eng = "dve" if wst["n"] % 2 == 0 else "pool"
        wst["n"] += 1
        sv = s_[:].rearrange("p a b -> p (a b)")[:, 0:DM]
        c.dma(sv, D["w_down"][L, k * 128:(k + 1) * 128, :], w=[(s_.name, 0), (s_.name, 1)])
        c.op(eng, lambda e: e.tensor_copy(out=wd[:, k, :], in_=sv), r=[(s_.name, 0), (s_.name, 1)], w=[("wd", k)])

    return {"P": P, "wgu": wgu, "wd": wd, "g2": g2, "load_gu": load_gu, "load_wd": load_wd}


def phase_E(c, L, D, xout, EW):
    K = c.K
    P = Pool_(c, "E%d" % L)
    wgu, wd, load_gu, load_wd = EW["wgu"], EW["wd"], EW["load_gu"], EW["load_wd"]
    xt = P.sb("xt", [128, DM], F32)
    xo2 = [P.sb("xo%d" % i, [128, DM], F32) for i in range(2)]
    junk = P.sb("junk", [128, DM], BF16)
    ss = [P.sb("ss%d" % i, [128, 1], F32) for i in range(2)]
    hb = [P.sb("hb%d" % i, [128, DM], BF16) for i in range(2)]
    hT = P.sb("hT", [128, 8, 512], BF16)
    sl = [P.sb("sl%d" % i, [128, 512], BF16) for i in range(2)]
    aT = P.sb("aT", [128, 22, 512], BF16)
    pT = [P.ps("pT%d" % i, [128, 8, 128], BF16) for i in range(2)]
    pO = P.ps("pO", [128, 2, 512], F32)
    pG = [P.ps("pG%d" % i, [128, 512], F32) for i in range(2)]
    pU = [P.ps("pU%d" % i, [128, 512], F32) for i in range(2)]
    for g in range(NG):
        for ti in range(4):
            it = g * 4 + ti
            t0 = it * 128
            pT_ = pT[it % 2]
            c.dma(xt[:], D["s_x1"][t0:t0 + 128, :], r=[("x1", it)])
            ss_, hb_ = ss[it % 2], hb[it % 2]
            c.op("act", lambda e: e.activation(out=junk[:], in_=xt[:], func=AF.Square, accum_out=ss_[:]), r=[xt], w=[junk, ss_])
            rsqrt_ops(c, ss_[:], ss_[:], 1.0 / DM, [ss_], [ss_])
            c.op("dve", lambda e: e.tensor_scalar(out=hb_[:], in0=xt[:], scalar1=ss_[:, 0:1], scalar2=None, op0=ALU.mult), r=[xt, ss_], w=[hb_])
            for k in range(8):
                c.op("pe", lambda e: e.transpose(out=pT_[:, k, :], in_=hb_[:, k * 128:(k + 1) * 128], identity=K["identb"][:]), r=[hb_, K["identb"]], w=[pT_])
            c.op("act", lambda e: e.copy(out=hT[:, :, ti * 128:(ti + 1) * 128], in_=pT_[:]), r=[pT_], w=[hT])
        for fc in range(22):
            pG_, pU_, sl_ = pG[fc % 2], pU[fc % 2], sl[fc % 2]
            if g == 0:
                load_gu()
                load_wd()
            for k in range(8):
                c.op("pe", lambda e: e.matmul(pG_[:], lhsT=wgu[:, fc, k, 0:128], rhs=hT[:, k, :], start=(k == 0), stop=(k == 7)),
                     r=[hT, ("wgu", fc)], w=[pG_])
            for k in range(8):
                c.op("pe", lambda e: e.matmul(pU_[:], lhsT=wgu[:, fc, k, 128:256], rhs=hT[:, k, :], start=(k == 0), stop=(k == 7)),
                     r=[hT, ("wgu", fc)], w=[pU_])
            c.op("act", lambda e: e.activation(out=sl_[:], in_=pG_[:], func=AF.Silu), r=[pG_], w=[sl_])
            c.op("dve", lambda e: e.tensor_tensor(out=aT[:, fc, :], in0=sl_[:], in1=pU_[:], op=ALU.mult), r=[sl_, pU_], w=[("aT", fc)])
        for ti in range(4):
            it = g * 4 + ti
            t0 = it * 128
            xo = xo2[it % 2]
            c.dma(xo[:], D["s_x1"][t0:t0 + 128, :], r=[("x1", it)])
            for hf in range(2):
                for fc in range(22):
                    c.op("pe", lambda e: e.matmul(pO[:, hf, :], lhsT=aT[:, fc, ti * 128:(ti + 1) * 128], rhs=wd[:, fc, hf * 512:(hf + 1) * 512], start=(fc == 0), stop=(fc == 21)),
                         r=[("aT", fc), ("wd", fc)], w=[("pO", hf)])
            c.op("dve", lambda e: e.tensor_tensor(out=xo[:], in0=xo[:], in1=pO[:].rearrange("p a b -> p (a b)"), op=ALU.add),
                 r=[xo, ("pO", 0), ("pO", 1)], w=[xo])
            c.dma(xout[t0:t0 + 128, :], xo[:], q="pool", w=[("x", it)])
    P.close()
    EW["P"].es.close()


def build_program(phases=("A", "B", "C", "D", "E"), depth=DEPTH, debug_out=(), ext_in=()):
    nc = bass.Bass("TRN2", target_bir_lowering=False)
    D = {}
    D["x"] = nc.dram_tensor("x", [T, DM], F32, kind="ExternalInput").ap()
    for k, shp in WEIGHT_SHAPES.items():
        D[k] = nc.dram_tensor(k, shp, F32, kind="ExternalInput").ap()
    for k, shp in CONST_SHAPES.items():
        D[k] = nc.dram_tensor(k, shp, F32, kind="ExternalInput").ap()
    D["out"] = nc.dram_tensor("out", [T, DM], F32, kind="ExternalOutput").ap()
    for k, (shp, dt) in SCRATCH.items():
        kind = "ExternalOutput" if k in debug_out else ("ExternalInput" if k in ext_in else "Internal")
        D[k] = nc.dram_tensor(k, shp, dt, kind=kind).ap()
    with ExitStack() as es:
        c = Ctx(nc, es)
        c.debug = len(debug_out) > 0
        KP = Pool_(c, "K")
        K = {}
        c.K = K
        c.eps_t = KP.sb("eps", [128, 1], F32)
        c.eps_ap = c.eps_t[:]
        c.op("pool", lambda e: e.memset(c.eps_t[:], EPS), w=[c.eps_t])
        c.one_t = KP.sb("one", [128, 1], F32)
        c.op("pool", lambda e: e.memset(c.one_t[:], 1.0), w=[c.one_t])
        cst = KP.sb("cstage", [128, 128], F32)
        for nm, src in (("identb", "c_ident"), ("bonesb", "c_bones"), ("trib", "c_tri"), ("lowb", "c_low")):
            K[nm] = KP.sb(nm, [128, 128], BF16)
            c.dma(cst[:], D[src])
            c.op("dve", lambda e: e.tensor_copy(out=K[nm][:], in_=cst[:]), r=[cst], w=[K[nm]])
        K["identf"] = KP.sb("identf", [128, 128], F32)
        c.dma(K["identf"][:], D["c_ident"])
        xin = D["x"]
        for L in range(depth):
            last = (L == depth - 1)
            xout = D["out"] if last else D["s_x2"]
            if "A" in phases:
                phase_A(c, L, D, xin)
            if "B" in phases:
                phase_B(c, L, D)
            if "C" in phases:
                phase_C(c, L, D)
            EW = phase_E_weights(c, L, D) if "E" in phases else None
            if "D" in phases:
                phase_D(c, L, D, xin, EW)
            if "E" in phases:
                phase_E(c, L, D, xout, EW)
            xin = xout
        c.barrier(["sp"])
        print("instructions", c.n_ins, "waits", c.n_wait)
    return nc


def phase_B(c, L, D):
    K = c.K
    P = Pool_(c, "B%d" % L)
    identb, trib, lowb = K["identb"], K["trib"], K["lowb"]
    KA = [P.sb("KA%d" % g, [128, T], BF16) for g in range(2)]
    KW = P.sb("KW", [64, 2, T], BF16)
    VSa = P.sb("VSa", [128, 2, 32, 65], BF16)
    VWa = P.sb("VWa", [128, 2, 32, 65], BF16)
    KcT = P.sb("KcT", [64, 2, 256], BF16)
    Ra = P.sb("Ra", [128, 2, 2, 129], BF16)
    M0b = P.sb("M0b", [128, 2560], BF16)
    gout = P.sb("gout", [128, 512], F32)
    c.dma(gout[:], D["nsa_out_norm_g"][L].partition_broadcast(128))
    P1 = Pool_(c, "B1_%d" % L)
    estg = P1.sb("estg", [128, T], F32)
    c.dma(estg[64:128, :], D["c_E"])
    for g in range(2):
        c.op("dve" if g == 0 else "pool", lambda e: e.tensor_copy(out=KA[g][64:128, :], in_=estg[64:128, :]), r=[estg], w=[KA[g]])
        c.dma(KA[g][0:64, :], D["s_ks"][g], r=[("s_ks", g, i) for i in range(NG)], w=[KA[g]])
        c.dma(KW[:, g, :], D["s_kw"][g], r=[("s_kw", g, i) for i in range(NG)], w=[KW])
        c.dma(VSa[:, g, :, 0:64], D["s_vs"][:, g * 64:(g + 1) * 64].rearrange("(kt p) d -> p kt d", p=128), r=[("s_vs", i) for i in range(32)], w=[VSa])
        c.dma(VWa[:, g, :, 0:64], D["s_vw"][:, g * 64:(g + 1) * 64].rearrange("(kt p) d -> p kt d", p=128), r=[("s_vw", i) for i in range(32)], w=[VWa])
    c.op("pool", lambda e: e.memset(VSa[:, :, :, 64:65], 1.0), r=[VSa], w=[VSa])
    c.op("pool", lambda e: e.memset(VWa[:, :, :, 64:65], 1.0), r=[VWa], w=[VWa])
    m0s = P1.sb("m0s", [128, 2560], F32)
    c.dma(m0s[:], D["c_m0"])
    c.op("dve", lambda e: e.tensor_copy(out=M0b[:], in_=m0s[:]), r=[m0s], w=[M0b])
    ovs = P1.sb("ovs", [128, 2, 64], F32)
    c.dma(ovs[:], D["c_ov"].rearrange("(c p) j -> p c j", p=128))
    c.op("pool", lambda e: e.memset(Ra[:], 0.0), w=[Ra])
    for g in range(2):
        c.op("dve", lambda e: e.tensor_copy(out=Ra[:, g, :, 65:129], in_=ovs[:]), r=[ovs, Ra], w=[Ra])
        c.op("pool", lambda e: e.memset(Ra[:, g, :, 64:65], 1.0), r=[Ra], w=[Ra])
    kg0 = P1.sb("kg0", [128, 64], F32)
    c.dma(kg0[:], D["nsa_k_norm_g"][L, 0].partition_broadcast(128))
    X2 = [P1.sb("X2_%d" % i, [128, T], BF16) for i in range(2)]
    w1s = P1.sb("w1s", [128, 16, 128], F32)
    w1b = [P1.sb("w1b%d" % i, [128, 16, 128], BF16) for i in range(2)]
    pes = P1.sb("pes", [128, 16], F32)
    peb = [P1.sb("peb%d" % i, [128, 16], BF16) for i in range(2)]
    w2s = P1.sb("w2s", [128, 64], F32)
    w2b = [P1.sb("w2b%d" % i, [128, 64], BF16) for i in range(2)]
    hbias = [P1.sb("hbias%d" % i, [128, 1], F32) for i in range(2)]
    hid = [P1.sb("hid%d" % i, [128, 256], BF16) for i in range(2)]
    kcf = P1.sb("kcf", [128, 64], F32)
    kjunk = P1.sb("kjunk", [128, 64], F32)
    kss = P1.sb("kss", [128, 1], F32)
    kcb = P1.sb("kcb", [128, 64], BF16)
    pH = P1.ps("pH", [128, 256], F32)
    pHb = P1.ps("pHb", [128, 1], F32)
    pK = P1.ps("pK", [128, 64], F32)
    pKT = P1.ps("pKT", [64, 128], BF16)
    for wi in range(2):
        c.dma(w1s[:], D["cmp_w1"][L, wi].rearrange("(l p) h -> p l h", p=128))
        c.op("dve", lambda e: e.tensor_copy(out=w1b[wi][:], in_=w1s[:]), r=[w1s], w=[w1b[wi]])
        c.dma(pes[:], D["cmp_pe"][L, wi].rearrange("l d -> (l d)").rearrange("(l p) -> p l", p=128), allow_slow_non_contiguous=True)
        c.op("dve", lambda e: e.tensor_copy(out=peb[wi][:], in_=pes[:]), r=[pes], w=[peb[wi]])
        c.dma(w2s[:], D["cmp_w2"][L, wi])
        c.op("dve", lambda e: e.tensor_copy(out=w2b[wi][:], in_=w2s[:]), r=[w2s], w=[w2b[wi]])
        for l in range(16):
            c.op("pe", lambda e: e.matmul(pHb[:], lhsT=w1b[wi][:, l, :], rhs=peb[wi][:, l:l + 1], start=(l == 0), stop=(l == 15)), r=[w1b[wi], peb[wi]], w=[pHb])
        c.op("act", lambda e: e.copy(out=hbias[wi][:], in_=pHb[:]), r=[pHb], w=[hbias[wi]])
        src = D["s_kc"] if wi == 0 else D["s_vc"]
        sk = "s_kc" if wi == 0 else "s_vc"
        for g in range(2):
            X = X2[g]
            c.dma(X[0:64, :], src[g * 64:(g + 1) * 64, 0:T], r=[(sk, i) for i in range(NG)], w=[X])
            c.dma(X[64:128, 0:T - 1], src[g * 64:(g + 1) * 64, 1:T], r=[(sk, i) for i in range(NG)], w=[X])
            hd = hid[g]
            for l in range(16):
                c.op("pe", lambda e: e.matmul(pH[:, 0:255], lhsT=w1b[wi][:, l, :], rhs=X[:, 2 * l:2 * l + 16 * 254 + 1:16], start=(l == 0), stop=(l == 15)),
                     r=[w1b[wi], X], w=[pH])
            c.op("pool", lambda e: e.memset(hd[:, 255:256], 0.0), w=[hd])
            c.op("act", lambda e: e.activation(out=hd[:, 0:255], in_=pH[:, 0:255], func=AF.Silu, bias=hbias[wi][:, 0:1]), r=[pH, hbias[wi], hd], w=[hd])
            for cch in range(2):
                rows = 128 if cch == 0 else 127
                c.op("pe", lambda e: e.matmul(pK[0:rows, :], lhsT=hd[:, cch * 128:cch * 128 + rows], rhs=w2b[wi][:], start=True, stop=True), r=[hd, w2b[wi]], w=[pK])
                if wi == 0:
                    c.op("act", lambda e: e.activation(out=kjunk[0:rows, :], in_=pK[0:rows, :], func=AF.Square, accum_out=kss[0:rows, :]), r=[pK], w=[kjunk, kss])
                    rsqrt_ops(c, kss[0:rows, :], kss[0:rows, :], 1.0 / 64, [kss], [kss])
                    c.op("dve", lambda e: e.scalar_tensor_tensor(out=kcb[0:rows, :], in0=pK[0:rows, :], scalar=kss[0:rows, 0:1], in1=kg0[0:rows, :], op0=ALU.mult, op1=ALU.mult),
                         r=[pK, kss, kg0], w=[kcb])
                    c.op("pe", lambda e: e.transpose(out=pKT[:, 0:rows], in_=kcb[0:rows, :], identity=identb[0:rows, 0:rows]), r=[kcb, identb], w=[pKT])
                    c.op("act", lambda e: e.copy(out=KcT[:, g, cch * 128:cch * 128 + rows], in_=pKT[:, 0:rows]), r=[pKT], w=[KcT])
                else:
                    c.op("act", lambda e: e.copy(out=Ra[0:rows, g, cch, 0:64], in_=pK[0:rows, :]), r=[pK, Ra], w=[Ra])
    P1.close()

    QA = [P.sb("QA%d" % i, [128, 8, 512], BF16) for i in range(2)]
    oacc = [P.sb("oacc%d" % i, [128, 4, 8, 64], F32) for i in range(2)]
    gat = [P.sb("gat%d" % i, [128, 4, 24], F32) for i in range(2)]
    bon = [P.sb("bon%d" % i, [128, 4, 64], F32) for i in range(2)]
    PT = [P.sb("PT%d" % i, [128, 512], BF16) for i in range(4)]
    impa = P.sb("impa", [128, 4, 64], F32)
    imt = P.sb("imt", [128, 4, 64], F32)
    rz = [P.sb("rz%d" % i, [128, 4], F32) for i in range(2)]
    gz = [P.sb("gz%d" % i, [128, 4], F32) for i in range(2)]
    otmp = [P.sb("otmp%d" % i, [128, 4, 64], F32) for i in range(2)]
    m8 = P.sb("m8", [128, 16], F32)
    sc2 = P.sb("sc2", [128, 64], F32)
    b16 = P.sb("b16", [128, 4, 64], BF16)
    bT = [P.sb("bT%d" % i, [64, 512], BF16) for i in range(2)]
    osq = P.sb("osq", [128, 4, 8, 64], F32)
    ossq = P.sb("ossq", [128, 32], F32)
    yab = [P.sb("yab%d" % i, [128, 4, 512], BF16) for i in range(2)]
    pS = [P.ps("pS%d" % i, [128, 512], F32) for i in range(3)]
    pOc = P.ps("pOc", [128, 4, 256], F32)
    pOs = P.ps("pOs", [128, 4, 65], F32)
    pOw = P.ps("pOw", [128, 4, 65], F32)
    pTk = P.ps("pTk", [64, 512], BF16)
    st = {"ns": 0, "npt": 0}

    def score_tile():
        st["ns"] += 1
        return pS[st["ns"] % 3]

    def pt_tile():
        st["npt"] += 1
        return PT[st["npt"] % 4]

    LA = 2
    stq = []

    def pop_tile():
        pS_, exp_fn, pv_fn, after = stq.pop(0)
        PT_ = pt_tile()
        exp_fn(pS_, PT_)
        pv_fn(PT_)
        if after is not None:
            after()

    def push_tile(score_fn, exp_fn, pv_fn, after=None):
        pS_ = score_tile()
        score_fn(pS_)
        stq.append((pS_, exp_fn, pv_fn, after))
        while len(stq) > LA:
            pop_tile()

    def B2(qg):
        par = qg % 2
        tg0 = qg * 512
        QA_, oacc_, gat_, bon_ = QA[par], oacc[par], gat[par], bon[par]
        c.dma(QA_[0:64, :, :], D["s_qa"][:, :, tg0:tg0 + 512].rearrange("h p t -> p h t"), r=[("s_qa", h, qg) for h in range(8)], w=[("QAq", par)])
        c.dma(gat_[:], D["s_ga"][tg0:tg0 + 512, :].rearrange("(s p) c -> p s c", p=128), r=[("s_ga", qg * 4 + i) for i in range(4)], w=[gat_])
        c.dma(bon_[:], D["c_bonus"][tg0:tg0 + 512, :].rearrange("(s p) j -> p s j", p=128), w=[bon_])

        def post_head(g, hh):
            h = g * 4 + hh
            rz_, gz_ = rz[h % 2], gz[h % 2]
            c.op("dve", lambda e: e.tensor_scalar(out=rz_[:], in0=pOc[:, :, 64], scalar1=1e-30, scalar2=None, op0=ALU.max), r=[pOc], w=[rz_])
            c.op("dve", lambda e: e.reciprocal(out=rz_[:], in_=rz_[:]), r=[rz_], w=[rz_])
            c.op("dve", lambda e: e.tensor_tensor(out=gz_[:], in0=rz_[:], in1=gat_[:, :, 3 * h], op=ALU.mult), r=[rz_, gat_], w=[gz_])
            tgt = impa if hh == 0 else imt
            c.op("dve", lambda e: e.tensor_tensor(out=tgt[:], in0=pOc[:, :, 65:129], in1=rz_[:].unsqueeze(2).to_broadcast([128, 4, 64]), op=ALU.mult),
                 r=[pOc, rz_], w=[tgt])
            if hh > 0:
                c.op("pool", lambda e: e.tensor_tensor(out=impa[:], in0=impa[:], in1=imt[:], op=ALU.add), r=[impa, imt], w=[impa])
            c.op("dve", lambda e: e.tensor_tensor(out=oacc_[:, :, h, :], in0=pOc[:, :, 0:64], in1=gz_[:].unsqueeze(2).to_broadcast([128, 4, 64]), op=ALU.mult),
                 r=[pOc, gz_], w=[("oacc", par, h)])
            if c.debug:
                c.dma(D["s_ocmp"][tg0:tg0 + 512, h * 64:(h + 1) * 64].rearrange("(s p) d -> p s d", p=128), oacc_[:, :, h, :], r=[("oacc", par, h)], w=[("dbg_ocmp", qg, h)])
            if hh == 3:
                topk(g)

        def topk(g):
            c.op("dve", lambda e: e.tensor_tensor(out=impa[:], in0=impa[:], in1=bon_[:], op=ALU.add), r=[impa, bon_], w=[impa])
            for sub in range(4):
                c.op("dve", lambda e: e.max(out=m8[:, 0:8], in_=impa[:, sub, :]), r=[impa], w=[m8])
                c.op("dve", lambda e: e.match_replace(out=sc2[:], in_to_replace=m8[:, 0:8], in_values=impa[:, sub, :], imm_value=-3e38), r=[impa, m8], w=[sc2])
                c.op("dve", lambda e: e.max(out=m8[:, 8:16], in_=sc2[:]), r=[sc2, m8], w=[m8])
                c.op("dve", lambda e: e.tensor_scalar(out=b16[:, sub, :], in0=impa[:, sub, :], scalar1=m8[:, 15:16], scalar2=NEG, op0=ALU.is_lt, op1=ALU.mult),
                     r=[impa, m8], w=[b16])
            for sub in range(4):
                c.op("pe", lambda e: e.transpose(out=pTk[:, sub * 128:(sub + 1) * 128], in_=b16[:, sub, :], identity=identb[:]), r=[b16, identb], w=[pTk])
            bT_ = bT[g]
            c.op("act", lambda e: e.copy(out=bT_[:], in_=pTk[:]), r=[pTk], w=[bT_])
            for hh in range(4):
                c.dma(QA_[64:128, g * 4 + hh, :], bT_[:], r=[bT_], w=[("QAb", par, g * 4 + hh)])
            if c.debug:
                c.dma(D["s_biasT"][g, :, tg0:tg0 + 512], bT_[:], r=[bT_], w=[("dbg_bT", qg, g)])

        for g in range(2):
            for hh in range(4):
                h = g * 4 + hh
                chunks = [0] if qg < 4 else [0, 1]
                for ci_, cch in enumerate(chunks):
                    rows = 128 if cch == 0 else 127
                    masked = (cch == 0 and qg <= 4) or (cch == 1)

                    def score(pS_, g=g, h=h, cch=cch, rows=rows, masked=masked):
                        c.op("pe", lambda e: e.matmul(pS_[0:rows, :], lhsT=KcT[:, g, cch * 128:cch * 128 + rows], rhs=QA_[0:64, h, :], start=True, stop=not masked),
                             r=[KcT, ("QAq", par)], w=[pS_])
                        if masked:
                            mc = 512 * qg if cch == 0 else 512 * (qg - 4)
                            c.op("pe", lambda e: e.matmul(pS_[0:rows, :], lhsT=identb[0:rows, 0:rows], rhs=M0b[0:rows, mc:mc + 512], start=False, stop=True),
                                 r=[identb, M0b], w=[pS_])

                    def expf(pS_, PT_, rows=rows):
                        c.op("act", lambda e: e.activation(out=PT_[0:rows, :], in_=pS_[0:rows, :], func=AF.Exp), r=[pS_], w=[PT_])

                    def pv(PT_, g=g, cch=cch, rows=rows, ci_=ci_, nch=len(chunks)):
                        for sub in range(4):
                            c.op("pe", lambda e: e.matmul(pOc[:, sub, 0:129], lhsT=PT_[0:rows, sub * 128:(sub + 1) * 128], rhs=Ra[0:rows, g, cch, :],
                                                          start=(ci_ == 0 and sub % 2 == 0), stop=(ci_ == nch - 1), skip_group_check=True), r=[PT_, Ra], w=[pOc])

                    last = (ci_ == len(chunks) - 1)
                    push_tile(score, expf, pv, after=(lambda g=g, hh=hh: post_head(g, hh)) if last else None)

    def B3(qg):
        par = qg % 2
        tg0 = qg * 512
        QA_, oacc_, gat_ = QA[par], oacc[par], gat[par]

        def post_branch(h, bi, pO_):
            rz_, gz_, ot_ = rz[bi % 2], gz[bi % 2], otmp[bi % 2]
            c.op("dve", lambda e: e.reciprocal(out=rz_[:], in_=pO_[:, :, 64]), r=[pO_], w=[rz_])
            c.op("dve", lambda e: e.tensor_tensor(out=gz_[:], in0=rz_[:], in1=gat_[:, :, 3 * h + bi], op=ALU.mult), r=[rz_, gat_], w=[gz_])
            c.op("dve", lambda e: e.tensor_tensor(out=ot_[:], in0=pO_[:, :, 0:64], in1=gz_[:].unsqueeze(2).to_broadcast([128, 4, 64]), op=ALU.mult), r=[pO_, gz_], w=[ot_])
            c.op("pool", lambda e: e.tensor_tensor(out=oacc_[:, :, h, :], in0=oacc_[:, :, h, :], in1=ot_[:], op=ALU.add), r=[("oacc", par, h), ot_], w=[("oacc", par, h)])
            if c.debug:
                dn_ = "s_dsel" if bi == 1 else "s_dwin"
                c.dma(D[dn_][tg0:tg0 + 512, h * 64:(h + 1) * 64].rearrange("(s p) d -> p s d", p=128), ot_[:], r=[ot_], w=[(dn_, qg, h)])
            if bi == 2 and h == 7:
                finish()

        def finish():
            ok = [("oacc", par, h) for h in range(8)]
            c.op("pool", lambda e: e.tensor_tensor(out=osq[:], in0=oacc_[:], in1=oacc_[:], op=ALU.mult), r=ok, w=[osq])
            c.op("dve", lambda e: e.tensor_reduce(out=ossq[:], in_=osq[:].rearrange("p s h d -> p (s h) d"), axis=AX.X, op=ALU.add), r=[osq], w=[ossq])
            rsqrt_ops(c, ossq[:], ossq[:], 1.0 / 64, [ossq], [ossq])
            c.op("dve", lambda e: e.tensor_tensor(out=osq[:].rearrange("p s h d -> p (s h) d"), in0=oacc_[:].rearrange("p s h d -> p (s h) d"),
                                                  in1=ossq[:].unsqueeze(2).to_broadcast([128, 32, 64]), op=ALU.mult), r=ok + [ossq], w=[osq])
            ya_ = yab[par]
            c.op("pool", lambda e: e.tensor_tensor(out=ya_[:], in0=osq[:].rearrange("p s h d -> p s (h d)"), in1=gout[:].unsqueeze(1).to_broadcast([128, 4, 512]), op=ALU.mult),
                 r=[osq, gout], w=[ya_])
            c.dma(D["s_ya"][tg0:tg0 + 512, :].rearrange("(s p) c -> p s c", p=128), ya_[:], q="pool", w=[("s_ya", qg * 4 + i) for i in range(4)])

        for h in range(8):
            g = h // 4
            qkeys = [("QAq", par), ("QAb", par, h)]
            nk = 4 * qg + 4
            for kt in range(nk):
                j = kt - 4 * qg
                c0 = 0 if j < 0 else 128 * j

                def score(pS_, g=g, h=h, kt=kt, j=j, c0=c0):
                    kcols = slice(kt * 128, (kt + 1) * 128)
                    if j < 0:
                        c.op("pe", lambda e: e.matmul(pS_[:, :], lhsT=KA[g][:, kcols], rhs=QA_[:, h, :], start=True, stop=True), r=[KA[g]] + qkeys, w=[pS_])
                    else:
                        c.op("pe", lambda e: e.matmul(pS_[:, c0:c0 + 128], lhsT=identb[:], rhs=trib[:], start=True, stop=False), r=[identb, trib], w=[pS_])
                        c.op("pe", lambda e: e.matmul(pS_[:, c0:c0 + 128], lhsT=KA[g][:, kcols], rhs=QA_[:, h, c0:c0 + 128], start=False, stop=True), r=[KA[g]] + qkeys, w=[pS_])
                        if c0 + 128 < 512:
                            c.op("pe", lambda e: e.matmul(pS_[:, c0 + 128:512], lhsT=KA[g][:, kcols], rhs=QA_[:, h, c0 + 128:512], start=True, stop=True), r=[KA[g]] + qkeys, w=[pS_])

                def expf(pS_, PT_, c0=c0):
                    c.op("act", lambda e: e.activation(out=PT_[:, c0:512], in_=pS_[:, c0:512], func=AF.Exp), r=[pS_], w=[PT_])

                def pv(PT_, g=g, kt=kt, c0=c0):
                    for sub in range(c0 // 128, 4):
                        c.op("pe", lambda e: e.matmul(pOs[:, sub, :], lhsT=PT_[:, sub * 128:(sub + 1) * 128], rhs=VSa[:, g, kt, :], start=(kt == 0 and sub == 0), stop=(kt == 4 * qg + sub), skip_group_check=True),
                             r=[PT_, VSa], w=[pOs])

                push_tile(score, expf, pv, after=(lambda h=h: post_branch(h, 1, pOs)) if kt == nk - 1 else None)
            tiles = [("lo", j) for j in range(4) if qg > 0] + [("up", j) for j in range(4)]
            for idx_, (kind, j) in enumerate(tiles):
                kt = 4 * qg - 4 + j if kind == "lo" else 4 * qg + j
                if kind == "lo":
                    c0, c1 = 0, 128 * (j + 1)
                    subs = list(range(0, j + 1))
                else:
                    c0, c1 = 128 * j, 512
                    subs = list(range(j, 4))

                def score(pS_, g=g, h=h, kt=kt, j=j, kind=kind, c0=c0):
                    kcols = slice(kt * 128, (kt + 1) * 128)
                    if kind == "lo":
                        m0_ = 128 * j
                        if j > 0:
                            c.op("pe", lambda e: e.matmul(pS_[:, 0:m0_], lhsT=KW[:, g, kcols], rhs=QA_[0:64, h, 0:m0_], start=True, stop=True), r=[KW, ("QAq", par)], w=[pS_])
                        c.op("pe", lambda e: e.matmul(pS_[:, m0_:m0_ + 128], lhsT=identb[:], rhs=lowb[:], start=True, stop=False), r=[identb, lowb], w=[pS_])
                        c.op("pe", lambda e: e.matmul(pS_[:, m0_:m0_ + 128], lhsT=KW[:, g, kcols], rhs=QA_[0:64, h, m0_:m0_ + 128], start=False, stop=True), r=[KW, ("QAq", par)], w=[pS_])
                    else:
                        c.op("pe", lambda e: e.matmul(pS_[:, c0:c0 + 128], lhsT=identb[:], rhs=trib[:], start=True, stop=False), r=[identb, trib], w=[pS_])
                        c.op("pe", lambda e: e.matmul(pS_[:, c0:c0 + 128], lhsT=KW[:, g, kcols], rhs=QA_[0:64, h, c0:c0 + 128], start=False, stop=True), r=[KW, ("QAq", par)], w=[pS_])
                        if c0 + 128 < 512:
                            c.op("pe", lambda e: e.matmul(pS_[:, c0 + 128:512], lhsT=KW[:, g, kcols], rhs=QA_[0:64, h, c0 + 128:512], start=True, stop=True), r=[KW, ("QAq", par)], w=[pS_])

                def expf(pS_, PT_, c0=c0, c1=c1):
                    c.op("act", lambda e: e.activation(out=PT_[:, c0:c1], in_=pS_[:, c0:c1], func=AF.Exp), r=[pS_], w=[PT_])

                def pv(PT_, g=g, kt=kt, kind=kind, j=j, subs=subs, first_tile=(idx_ == 0)):
                    for sub in subs:
                        first = first_tile and sub == subs[0]
                        last = (kind == "up" and j == sub)
                        c.op("pe", lambda e: e.matmul(pOw[:, sub, :], lhsT=PT_[:, sub * 128:(sub + 1) * 128], rhs=VWa[:, g, kt, :], start=first, stop=last, skip_group_check=True),
                             r=[PT_, VWa], w=[pOw])

                push_tile(score, expf, pv, after=(lambda h=h: post_branch(h, 2, pOw)) if idx_ == len(tiles) - 1 else None)

    for step in range(NG + 1):
        if step < NG:
            B2(step)
        if step >= 1:
            B3(step - 1)
            while stq:
                pop_tile()
    P.close()


def phase_C(c, L, D):
    K = c.K
    identf, identb = K["identf"], K["identb"]
    P = Pool_(c, "C%d" % L)
    GT = P.sb("GT", [128, 32, 3, 4], F32)
    decb = P.sb("decb", [128, 256], F32)
    P0 = Pool_(c, "C0_%d" % L)
    ifT = P0.sb("ifT", [128, 32, 8], F32)
    c.dma(ifT[:], D["s_if"].rearrange("(i p) c -> p i c", p=128), r=[("s_if", i) for i in range(32)])
    fb = P0.sb("fb", [4, 1], F32)
    c.dma(fb[:], D["m_fgate_b"][L].rearrange("(p o) -> p o", o=1))
    c.op("dve", lambda e: e.tensor_scalar(out=fb[:], in0=fb[:], scalar1=-1.0, scalar2=None, op0=ALU.mult), r=[fb], w=[fb])
    G = {n: P0.sb("G" + n, [4, T], F32) for n in ("I", "F", "X1", "X2", "KM", "RM", "CM", "NM")}
    pG = [P0.ps("pG%d" % i, [4, 4, 128], F32) for i in range(2)]
    n = 0
    for col, dst in ((0, G["I"]), (4, G["F"])):
        for i4 in range(8):
            pg = pG[n % 2]
            n += 1
            for j in range(4):
                it = i4 * 4 + j
                c.op("pe", lambda e: e.transpose(out=pg[:, j, :], in_=ifT[:, it, col:col + 4], identity=identf[:]), r=[ifT, identf], w=[pg])
            c.op("act", lambda e: e.copy(out=dst[:, i4 * 512:(i4 + 1) * 512], in_=pg[:].rearrange("p a b -> p (a b)")), r=[pg], w=[dst])
    c.op("pool", lambda e: e.memset(G["KM"][:], 1.0), w=[G["KM"]])
    c.op("pool", lambda e: e.memset(G["KM"][:, 0:T:64], 0.0), r=[G["KM"]], w=[G["KM"]])
    c.op("pool", lambda e: e.memset(G["RM"][:], 0.0), w=[G["RM"]])
    c.op("pool", lambda e: e.memset(G["RM"][:, 0:T:64], -1e30), r=[G["RM"]], w=[G["RM"]])
    c.op("act", lambda e: e.activation(out=G["X1"][:], in_=G["F"][:], func=AF.Exp, scale=-1.0, bias=fb[:, 0:1]), r=[G["F"], fb], w=[G["X1"]])
    c.op("act", lambda e: e.activation(out=G["X1"][:], in_=G["X1"][:], func=AF.Ln, bias=c.one_t[0:4, :]), r=[G["X1"], c.one_t], w=[G["X1"]])
    c.op("dve", lambda e: e.tensor_tensor_scan(out=G["X2"][:], data0=G["KM"][:], data1=G["X1"][:], initial=0.0, op0=ALU.mult, op1=ALU.add),
         r=[G["KM"], G["X1"]], w=[G["X2"]])
    c.op("dve", lambda e: e.tensor_tensor(out=G["I"][:], in0=G["I"][:], in1=G["X2"][:], op=ALU.add), r=[G["I"], G["X2"]], w=[G["I"]])
    c.op("dve", lambda e: e.tensor_tensor_scan(out=G["CM"][:], data0=G["RM"][:], data1=G["I"][:], initial=-1e30, op0=ALU.add, op1=ALU.max),
         r=[G["RM"], G["I"]], w=[G["CM"]])
    sm = {n: P0.sb("sm" + n, [4, 64], F32) for n in ("U", "B", "mn", "m", "d")}
    c.op("dve", lambda e: e.tensor_copy(out=sm["U"][:], in_=G["CM"][:, 63:T:64]), r=[G["CM"]], w=[sm["U"]])
    c.op("dve", lambda e: e.tensor_scalar(out=sm["B"][:], in0=G["X2"][:, 63:T:64], scalar1=-1.0, scalar2=None, op0=ALU.mult), r=[G["X2"]], w=[sm["B"]])
    c.op("dve", lambda e: e.tensor_tensor_scan(out=sm["mn"][:], data0=sm["U"][:], data1=sm["B"][:], initial=0.0, op0=ALU.max, op1=ALU.add),
         r=[sm["U"], sm["B"]], w=[sm["mn"]])
    c.op("dve", lambda e: e.memset(sm["m"][:, 0:1], 0.0), w=[sm["m"]])
    c.op("dve", lambda e: e.tensor_copy(out=sm["m"][:, 1:64], in_=sm["mn"][:, 0:63]), r=[sm["mn"], sm["m"]], w=[sm["m"]])
    v3 = lambda t_: t_[:].rearrange("p (c t) -> p c t", t=64)
    mb = sm["m"][:].unsqueeze(2).to_broadcast([4, 64, 64])
    c.op("dve", lambda e: e.tensor_tensor(out=v3(G["CM"]), in0=v3(G["CM"]), in1=mb, op=ALU.max), r=[G["CM"], sm["m"]], w=[G["CM"]])
    c.op("dve", lambda e: e.tensor_scalar(out=G["NM"][:], in0=G["CM"][:], scalar1=-1.0, scalar2=None, op0=ALU.mult), r=[G["CM"]], w=[G["NM"]])
    c.dma(D["s_mg"][0], G["NM"][:], w=[("s_mg", 0)])
    c.op("dve", lambda e: e.tensor_tensor(out=v3(G["X1"]), in0=v3(G["NM"]), in1=mb, op=ALU.add), r=[G["NM"], sm["m"], G["X1"]], w=[G["X1"]])
    c.op("act", lambda e: e.activation(out=G["F"][:], in_=G["X1"][:], func=AF.Exp), r=[G["X1"], G["F"]], w=[G["F"]])
    c.dma(D["s_mg"][1], G["F"][:], w=[("s_mg", 1)])
    c.op("dve", lambda e: e.tensor_copy(out=sm["d"][:], in_=G["F"][:, 63:T:64]), r=[G["F"]], w=[sm["d"]])
    c.dma(D["s_dec"], sm["d"][:], w=[("s_dec",)])
    c.dma(decb[:], D["s_dec"].rearrange("h c -> (h c)").partition_broadcast(128), r=[("s_dec",)], w=[decb])
    c.op("dve", lambda e: e.tensor_tensor(out=G["X1"][:], in0=G["X2"][:], in1=G["NM"][:], op=ALU.add), r=[G["X2"], G["NM"], G["X1"]], w=[G["X1"]])
    c.op("act", lambda e: e.activation(out=G["KM"][:], in_=G["X1"][:], func=AF.Exp), r=[G["X1"], G["KM"]], w=[G["KM"]])
    nm63 = G["NM"][:, 63:T:64].unsqueeze(2).to_broadcast([4, 64, 64])
    c.op("dve", lambda e: e.tensor_tensor(out=v3(G["X1"]), in0=v3(G["I"]), in1=nm63, op=ALU.add), r=[G["I"], G["NM"], G["X1"]], w=[G["X1"]])
    c.op("act", lambda e: e.activation(out=G["RM"][:], in_=G["X1"][:], func=AF.Exp), r=[G["X1"], G["RM"]], w=[G["RM"]])
    c.op("dve", lambda e: e.tensor_scalar(out=G["RM"][:], in0=G["RM"][:], scalar1=128.0 ** -0.5, scalar2=None, op0=ALU.mult), r=[G["RM"]], w=[G["RM"]])
    PK = P0.sb("PK", [128, T], F32)
    c.op("pool", lambda e: e.memset(PK[:], 0.0), w=[PK])
    for q, src in enumerate((G["I"], G["KM"], G["RM"])):
        c.dma(PK[32 * q:32 * q + 4, :], src[:], r=[src, PK], w=[PK])
    pP = [P0.ps("pP%d" % i, [128, 4, 128], F32) for i in range(2)]
    for i4 in range(8):
        pp = pP[i4 % 2]
        for j in range(4):
            it = i4 * 4 + j
            c.op("pe", lambda e: e.transpose(out=pp[:, j, :], in_=PK[:, it * 128:(it + 1) * 128], identity=identf[:]), r=[PK, identf], w=[pp])
        c.op("act", lambda e: e.copy(out=GT[:, i4 * 4:(i4 + 1) * 4, :, :], in_=pp[:, :, 0:96].rearrange("p j (q x) -> p j q x", x=32)[:, :, :, 0:4]), r=[pp, GT], w=[GT])
    P0.close()

    mBD = P.sb("mBD", [128, 128], F32)
    c.op("pool", lambda e: e.memset(mBD[:], -1e30), w=[mBD])
    c.dma(mBD[0:64, 0:64], D["c_mcaus"], r=[mBD], w=[mBD])
    c.dma(mBD[64:128, 64:128], D["c_mcaus"], r=[mBD], w=[mBD])
    gm = P.sb("gm", [128, 512], F32)
    c.dma(gm[:], D["m_out_norm_g"][L].partition_broadcast(128))
    nmb = [P.sb("nmb%d" % i, [128, 4, 512], F32) for i in range(2)]
    wib = [P.sb("wib%d" % i, [128, 4, 512], F32) for i in range(2)]
    vA = [P.sb("vA%d" % i, [128, 4, 4, 129], BF16) for i in range(2)]
    obt = [P.sb("obt%d" % i, [128, 4, 512], F32) for i in range(2)]
    ybt = [P.sb("ybt%d" % i, [128, 4, 512], BF16) for i in range(2)]
    qT = [[P.sb("qT%d_%d" % (i, h), [128, 512], BF16) for h in range(4)] for i in range(2)]
    kT = [[P.sb("kT%d_%d" % (i, h), [128, 512], BF16) for h in range(4)] for i in range(2)]
    q2 = [[P.sb("q2_%d_%d" % (i, h), [128, 2, 512], BF16) for h in range(4)] for i in range(2)]
    ksc = [[P.sb("ksc%d_%d" % (i, h), [128, 4, 128], BF16) for h in range(4)] for i in range(2)]
    wi = [[P.sb("wi%d_%d" % (i, h), [128, 4, 128], BF16) for h in range(4)] for i in range(2)]
    wt = [P.sb("wt%d" % i, [128, 4, 128], F32) for i in range(2)]
    Cf = [P.sb("Cf%d" % h, [128, 129], F32) for h in range(4)]
    Cb = [[P.sb("Cb%d_%d" % (h, i), [128, 129], BF16) for i in range(3)] for h in range(4)]
    ncb = [0, 0, 0, 0]
    nums = [P.sb("nums%d" % i, [128, 4, 4, 129], F32) for i in range(2)]
    psq = P.sb("psq", [128, 16, 128], F32)
    pd16 = [P.sb("pd16_%d" % i, [128, 16], F32) for i in range(4)]
    for i in range(2):
        for h in range(4):
            c.op("pool", lambda e: e.memset(q2[i][h][:], 0.0), w=[q2[i][h]])
        c.op("pool", lambda e: e.memset(vA[i][:, :, :, 128:129], 1.0), w=[vA[i]])
    for h in range(4):
        c.op("pool", lambda e: e.memset(Cf[h][:], 0.0), w=[Cf[h]])
        c.op("pool", lambda e: e.memset(Cb[h][0][:], 0.0), w=[Cb[h][0]])
    pSm = [P.ps("pSm%d" % i, [128, 4, 128], F32) for i in range(2)]
    pKt = [P.ps("pKt%d" % i, [128, 4, 128], BF16) for i in range(1)]
    pNm = [P.ps("pNm%d" % i, [128, 2, 129], F32) for i in range(2)]
    pDl = [P.ps("pDl%d" % i, [128, 2, 129], F32) for i in range(2)]
    cnt = {"sm": 0}

    def loads(cg):
        tg0 = cg * 512
        par = cg % 2
        nmb_, wib_, vA_, obt_ = nmb[par], wib[par], vA[par], obt[par]
        c.dma(nmb_[:], D["s_mg"][0, :, tg0:tg0 + 512].partition_broadcast(128), r=[("s_mg", 0)], w=[nmb_])
        c.dma(wib_[:], D["s_mg"][1, :, tg0:tg0 + 512].partition_broadcast(128), r=[("s_mg", 1)], w=[wib_])
        for ti in range(4):
            it = cg * 4 + ti
            c.dma(vA_[:, ti, :, 0:128], D["s_vb"][it * 128:(it + 1) * 128, :].rearrange("p (h e) -> p h e", e=128), r=[("s_vb", it)], w=[vA_])
        c.dma(obt_[:], D["s_ob"][tg0:tg0 + 512, :].rearrange("(s p) c -> p s c", p=128), r=[("s_ob", cg * 4 + i) for i in range(4)], w=[obt_])
        c.op("pool", lambda e: e.tensor_tensor(out=obt_[:], in0=obt_[:], in1=gm[:].unsqueeze(1).to_broadcast([128, 4, 512]), op=ALU.mult), r=[obt_, gm], w=[obt_])
        for h in range(4):
            c.dma(qT[par][h][:], D["s_qb"][h, :, tg0:tg0 + 512], r=[("s_qb", h, cg)])
            c.dma(kT[par][h][:], D["s_kb"][h, :, tg0:tg0 + 512], r=[("s_kb", h, cg)])

    def prep(cg, h):
        par = cg % 2
        nmb_, wib_ = nmb[par], wib[par]
        qT_, kT_, q2_, ksc_, wi_ = qT[par][h], kT[par][h], q2[par][h], ksc[par][h], wi[par][h]
        cnt["sm"] += 1
        pSm_, wt_, pKt_ = pSm[cnt["sm"] % 2], wt[cnt["sm"] % 2], pKt[0]
        for eo in range(2):
            vw = lambda a: a.rearrange("p (c two t) -> p c two t", two=2, t=64)[:, :, eo, :]
            c.op("dve", lambda e: e.tensor_tensor(out=vw(q2_[:, eo, :]), in0=vw(qT_[:]), in1=vw(wib_[:, h, :]), op=ALU.mult), r=[qT_, wib_, q2_], w=[q2_])
        for ti in range(4):
            cs = slice(ti * 128, (ti + 1) * 128)
            c.op("pe", lambda e: e.matmul(pSm_[:, ti, :], lhsT=kT_[:, cs], rhs=qT_[:, cs], start=True, stop=True), r=[kT_, qT_], w=[pSm_])
        for ti in range(4):
            cs = slice(ti * 128, (ti + 1) * 128)
            c.op("pe", lambda e: e.transpose(out=pKt_[:, ti, :], in_=kT_[:, cs], identity=identb[:]), r=[kT_, identb], w=[pKt_])
        for ti in range(4):
            it = cg * 4 + ti
            c.op("dve", lambda e: e.tensor_scalar(out=ksc_[:, ti, :], in0=pKt_[:, ti, :], scalar1=GT[:, it, 2, h:h + 1], scalar2=None, op0=ALU.mult), r=[pKt_, GT, ksc_], w=[ksc_])
        c.op("pool", lambda e: e.tensor_tensor(out=wt_[:], in0=nmb_[:, h, :].rearrange("p (j t) -> p j t", t=128), in1=mBD[:].unsqueeze(1).to_broadcast([128, 4, 128]), op=ALU.add),
             r=[nmb_, mBD], w=[wt_])
        for ti in range(4):
            it = cg * 4 + ti
            c.op("act", lambda e: e.activation(out=wt_[:, ti, :], in_=wt_[:, ti, :], func=AF.Exp, bias=GT[:, it, 0, h:h + 1]), r=[wt_, GT], w=[wt_])
        c.op("dve", lambda e: e.scalar_tensor_tensor(out=wi_[:].rearrange("p a b -> p (a b)"), in0=pSm_[:].rearrange("p a b -> p (a b)"), scalar=128.0 ** -0.5,
                                                     in1=wt_[:].rearrange("p a b -> p (a b)"), op0=ALU.mult, op1=ALU.mult), r=[wt_, pSm_], w=[wi_])

    def recur_tile(cg, ti):
        par = cg % 2
        vA_, nums_ = vA[par], nums[par]
        it = cg * 4 + ti
        cs = slice(ti * 128, (ti + 1) * 128)
        cbs = []
        for h in range(4):
            cbs.append((Cb[h][ncb[h] % 3], Cb[h][(ncb[h] + 1) % 3], Cb[h][(ncb[h] + 2) % 3]))
            ncb[h] += 2
        for half, (p0, p1) in enumerate(((0, 64), (64, 128))):
            ce = 2 * it + half
            for h in range(4):
                pd = pDl[h // 2]
                c.op("pe", lambda e: e.matmul(pd[:, h % 2, :], lhsT=ksc[par][h][p0:p1, ti, :], rhs=vA_[p0:p1, ti, h, :], start=True, stop=True),
                     r=[ksc[par][h], vA_], w=[(pd.name, h % 2)])
            for h in range(4):
                pd = pDl[h // 2]
                c.op("dve", lambda e: e.scalar_tensor_tensor(out=Cf[h][:], in0=Cf[h][:], scalar=decb[:, h * 64 + ce:h * 64 + ce + 1], in1=pd[:, h % 2, :], op0=ALU.mult, op1=ALU.add),
                     r=[Cf[h], decb, (pd.name, h % 2)], w=[Cf[h]])
                tgt = cbs[h][1] if half == 0 else cbs[h][2]
                c.op("act", lambda e: e.copy(out=tgt[:], in_=Cf[h][:]), r=[Cf[h]], w=[tgt])
            if half == 0:
                for h in range(4):
                    pn = pNm[h // 2]
                    q2_, wi_ = q2[par][h], wi[par][h]
                    cb_e, cb_o, _ = cbs[h]
                    c.op("pe", lambda e: e.matmul(pn[:, h % 2, :], lhsT=q2_[:, 0, cs], rhs=cb_e[:], start=True, stop=False), r=[q2_, cb_e], w=[(pn.name, h % 2)])
                    c.op("pe", lambda e: e.matmul(pn[:, h % 2, :], lhsT=q2_[:, 1, cs], rhs=cb_o[:], start=False, stop=False), r=[q2_, cb_o], w=[(pn.name, h % 2)])
                    c.op("pe", lambda e: e.matmul(pn[:, h % 2, :], lhsT=wi_[:, ti, :], rhs=vA_[:, ti, h, :], start=False, stop=True), r=[wi_, vA_], w=[(pn.name, h % 2)])
        for hp in range(2):
            pn = pNm[hp]
            c.op("act", lambda e: e.copy(out=nums_[:, ti, 2 * hp:2 * hp + 2, :], in_=pn[:]), r=[(pn.name, 0), (pn.name, 1)], w=[("nums", par, ti, hp)])

    def post(cg):
        par = cg % 2
        tg0 = cg * 512
        nums_, obt_, ybt_ = nums[par], obt[par], ybt[par]
        nk = [("nums", par, ti, hp) for ti in range(4) for hp in range(2)]
        v16 = lambda t_: t_[:].rearrange("p (a b) -> p a b", b=4)
        d0, d1, d2, d3 = pd16
        c.op("act", lambda e: e.activation(out=v16(d0), in_=nums_[:, :, :, 128], func=AF.Abs), r=nk, w=[d0])
        c.op("dve", lambda e: e.tensor_tensor(out=v16(d0), in0=v16(d0), in1=GT[:, cg * 4:(cg + 1) * 4, 1, :], op=ALU.max), r=[d0, GT], w=[d0])
        c.op("dve", lambda e: e.reciprocal(out=d0[:], in_=d0[:]), r=[d0], w=[d0])
        nv = nums_[:].rearrange("p a b e -> p (a b) e")[:, :, 0:128]
        c.op("pool", lambda e: e.tensor_tensor(out=psq[:], in0=nv, in1=nv, op=ALU.mult), r=nk, w=[psq])
        c.op("dve", lambda e: e.tensor_reduce(out=d1[:], in_=psq[:], axis=AX.X, op=ALU.add), r=[psq], w=[d1])
        c.op("dve", lambda e: e.tensor_tensor(out=d1[:], in0=d1[:], in1=d0[:], op=ALU.mult), r=[d1, d0], w=[d1])
        c.op("dve", lambda e: e.tensor_tensor(out=d1[:], in0=d1[:], in1=d0[:], op=ALU.mult), r=[d1, d0], w=[d1])
        rsqrt_ops(c, d2[:], d1[:], 1.0 / 128, [d1], [d2])
        c.op("dve", lambda e: e.tensor_tensor(out=d3[:], in0=d0[:], in1=d2[:], op=ALU.mult), r=[d0, d2], w=[d3])
        c.op("dve", lambda e: e.tensor_tensor(out=psq[:], in0=nv, in1=d3[:].unsqueeze(2).to_broadcast([128, 16, 128]), op=ALU.mult), r=nk + [d3, psq], w=[psq])
        c.op("pool", lambda e: e.tensor_tensor(out=ybt_[:], in0=psq[:].rearrange("p (a b) e -> p a (b e)", b=4), in1=obt_[:], op=ALU.mult), r=[psq, obt_], w=[ybt_])
        c.dma(D["s_yb"][tg0:tg0 + 512, :].rearrange("(s p) c -> p s c", p=128), ybt_[:], q="pool", w=[("s_yb", cg * 4 + i) for i in range(4)])

    loads(0)
    for h in range(4):
        prep(0, h)
    for cg in range(NG):
        if cg + 1 < NG:
            loads(cg + 1)
        for ti in range(4):
            recur_tile(cg, ti)
            if cg + 1 < NG:
                prep(cg + 1, ti)
        post(cg)
    P.close()


_CONSTS = None


def kernel(**inputs):
    global _CONSTS
    if _CONSTS is None:
        _CONSTS = host_consts()
    nc = build_program()
    x = np.ascontiguousarray(inputs["x"], dtype=np.float32)
    base = {k: np.ascontiguousarray(inputs[k], dtype=np.float32) for k in WEIGHT_SHAPES}
    base.update(_CONSTS)
    in_maps = []
    for b in range(8):
        m = dict(base)
        m["x"] = x[b]
        in_maps.append(m)
    res = run_bass_kernel_spmd(nc, in_maps, core_ids=list(range(8)))
    return np.stack([res.results[b]["out"] for b in range(8)], axis=0).astype(np.float32)
```
